# Optimizing a Trainium2 kernel written in Bass

```python
import jax, jax.numpy as jnp
from jax import lax
import numpy as np

D_MODEL = 1024
BATCH = 4
SEQ = 8192
DEPTH = 2

D_MIX = 2 * D_MODEL
D_SSD = D_MIX // 2
SSD_HEAD_DIM = 64
SSD_HEADS = D_SSD // SSD_HEAD_DIM
SSD_GROUPS = 2
SSD_HEADS_PER_GROUP = SSD_HEADS // SSD_GROUPS
SSD_STATE = 128
SSD_CHUNK = 128
CONV_WIDTH = 4
D_CONV = D_SSD + 2 * SSD_GROUPS * SSD_STATE
D_SGU = D_MIX - D_SSD
SGU_HEAD_DIM = 128
SGU_HEADS = D_SGU // SGU_HEAD_DIM
SGU_CHUNK = 128
D_IN_PROJ = D_SSD + D_CONV + SSD_HEADS + 2 * D_SGU
D_FF = 3584
N_EXPERTS = 8
TOP_K = 2
N_DENSE = (DEPTH + 1) // 2
N_MOE = DEPTH // 2
EPS = 1e-6

kernel_name = 'hymba_style_ssd_gmlp_moe_trunk'


def rms_norm(x, g):
    xf = x.astype(jnp.float32)
    y = xf * lax.rsqrt(jnp.mean(xf * xf, axis=-1, keepdims=True) + EPS)
    return (y * g.astype(jnp.float32)).astype(x.dtype)


def layer_norm(x, g, b):
    xf = x.astype(jnp.float32)
    mu = jnp.mean(xf, axis=-1, keepdims=True)
    var = jnp.mean(jnp.square(xf - mu), axis=-1, keepdims=True)
    y = (xf - mu) * lax.rsqrt(var + EPS) * g.astype(jnp.float32) + b.astype(jnp.float32)
    return y.astype(x.dtype)


def causal_depthwise_conv(x, w, b):
    out = lax.conv_general_dilated(
        x, w[:, None, :], window_strides=(1,), padding=[(CONV_WIDTH - 1, 0)],
        dimension_numbers=('NWC', 'WIO', 'NWC'), feature_group_count=x.shape[-1])
    return out + b


def ssd_chunked(x, dt, a, b_mat, c_mat):
    bsz, s, _, _ = x.shape
    nc = s // SSD_CHUNK
    xr = x.reshape(bsz, nc, SSD_CHUNK, SSD_GROUPS, SSD_HEADS_PER_GROUP, SSD_HEAD_DIM)
    dtr = dt.reshape(bsz, nc, SSD_CHUNK, SSD_GROUPS, SSD_HEADS_PER_GROUP)
    br = b_mat.reshape(bsz, nc, SSD_CHUNK, SSD_GROUPS, SSD_STATE)
    cr = c_mat.reshape(bsz, nc, SSD_CHUNK, SSD_GROUPS, SSD_STATE)
    da = dtr * a.reshape(SSD_GROUPS, SSD_HEADS_PER_GROUP)
    da_cs = jnp.cumsum(da, axis=2)
    xdt = xr * dtr[..., None]

    idx = jnp.arange(SSD_CHUNK)
    causal = (idx[:, None] >= idx[None, :])[:, :, None, None]
    seg = da_cs[:, :, :, None] - da_cs[:, :, None, :]
    decay = jnp.exp(jnp.where(causal, seg, -jnp.inf))
    cb = jnp.einsum('bclgn,bcsgn->bclsg', cr, br)
    scores = cb[..., None] * decay
    y_diag = jnp.einsum('bclsgr,bcsgrp->bclgrp', scores, xdt)

    decay_to_end = jnp.exp(da_cs[:, :, -1:] - da_cs)
    chunk_states = jnp.einsum('bclgn,bclgrp->bcgrpn', br, xdt * decay_to_end[..., None])
    chunk_decay = jnp.exp(da_cs[:, :, -1])

    def step(h, inp):
        st, dec = inp
        return h * dec[..., None, None] + st, h

    h0 = jnp.zeros((bsz, SSD_GROUPS, SSD_HEADS_PER_GROUP, SSD_HEAD_DIM, SSD_STATE), x.dtype)
    _, prev = lax.scan(step, h0, (jnp.moveaxis(chunk_states, 1, 0), jnp.moveaxis(chunk_decay, 1, 0)))
    prev = jnp.moveaxis(prev, 0, 1)
    y_off = jnp.einsum('bclgn,bcgrpn->bclgrp', cr, prev) * jnp.exp(da_cs)[..., None]
    return (y_diag + y_off).reshape(bsz, s, SSD_HEADS, SSD_HEAD_DIM)


def hybrid_mixer(h, w_in, conv_w, conv_b, dt_bias, a_log, d_skip, ssd_norm_g,
                 sgu_norm_g, sgu_norm_b, w_spatial, b_spatial, sgu_out_g, w_out):
    bsz, s, _ = h.shape
    f32 = jnp.float32
    proj = h @ w_in
    z, xbc, dt, uv = jnp.split(proj, [D_SSD, D_SSD + D_CONV, D_SSD + D_CONV + SSD_HEADS], axis=-1)

    xbc = jax.nn.silu(causal_depthwise_conv(xbc, conv_w, conv_b))
    xs, bm, cm = jnp.split(xbc, [D_SSD, D_SSD + SSD_GROUPS * SSD_STATE], axis=-1)
    dt = jax.nn.softplus(dt.astype(f32) + dt_bias.astype(f32))
    a = -jnp.exp(a_log.astype(f32))
    xs_h = xs.reshape(bsz, s, SSD_HEADS, SSD_HEAD_DIM).astype(f32)
    y = ssd_chunked(xs_h, dt, a,
                    bm.reshape(bsz, s, SSD_GROUPS, SSD_STATE).astype(f32),
                    cm.reshape(bsz, s, SSD_GROUPS, SSD_STATE).astype(f32))
    y = y + d_skip.astype(f32)[:, None] * xs_h
    y = y.reshape(bsz, s, D_SSD) * jax.nn.silu(z.astype(f32))
    y_ssd = rms_norm(y, ssd_norm_g).astype(h.dtype)

    u, v = jnp.split(jax.nn.gelu(uv, approximate=False), 2, axis=-1)
    v = layer_norm(v, sgu_norm_g, sgu_norm_b)
    nc = s // SGU_CHUNK
    vr = v.reshape(bsz, nc, SGU_CHUNK, SGU_HEADS, SGU_HEAD_DIM)
    tril = jnp.tril(jnp.ones((SGU_CHUNK, SGU_CHUNK), dtype=bool))
    ws = jnp.where(tril, w_spatial, jnp.zeros_like(w_spatial))
    gate = jnp.einsum('hij,bcjhp->bcihp', ws, vr) + b_spatial.T[None, None, :, :, None]
    y_sgu = u * gate.reshape(bsz, s, D_SGU)
    y_sgu = rms_norm(y_sgu, sgu_out_g)

    return jnp.concatenate([y_ssd, y_sgu], axis=-1) @ w_out


def swiglu(h, wg, wu, wd):
    return (jax.nn.silu(h @ wg) * (h @ wu)) @ wd


def moe_swiglu(h, w_router, e_gate, e_up, e_down):
    logits = (h @ w_router).astype(jnp.float32)
    top_vals, top_idx = lax.top_k(logits, TOP_K)
    top_w = jax.nn.softmax(top_vals, axis=-1)
    gates = jnp.sum(jax.nn.one_hot(top_idx, N_EXPERTS, dtype=jnp.float32) * top_w[..., None], axis=-2)
    gates = gates.astype(h.dtype)
    out = jnp.zeros_like(h)
    for e in range(N_EXPERTS):
        out = out + gates[..., e:e + 1] * swiglu(h, e_gate[e], e_up[e], e_down[e])
    return out


def setup_inputs(seed: int = 0) -> dict:
    key = jax.random.key(seed)
    ks = jax.random.split(key, 24)
    f32 = jnp.float32

    def nrm(k, shape, scale):
        return jax.random.normal(k, shape, f32) * scale

    def gain(k, shape):
        return 1.0 + 0.02 * jax.random.normal(k, shape, f32)

    dt0 = jnp.exp(jax.random.uniform(ks[5], (DEPTH, SSD_HEADS), f32, np.log(1e-3), np.log(1e-1)))
    dt_bias = dt0 + jnp.log(-jnp.expm1(-dt0))
    a_log = jnp.log(jax.random.uniform(ks[6], (DEPTH, SSD_HEADS), f32, 1.0, 16.0))

    return {
        'x': jax.random.normal(ks[0], (BATCH, SEQ, D_MODEL), f32),
        'norm_mix_g': gain(ks[1], (DEPTH, D_MODEL)),
        'w_in': nrm(ks[2], (DEPTH, D_MODEL, D_IN_PROJ), D_MODEL ** -0.5),
        'conv_w': nrm(ks[3], (DEPTH, CONV_WIDTH, D_CONV), CONV_WIDTH ** -0.5),
        'conv_b': nrm(ks[4], (DEPTH, D_CONV), 0.02),
        'dt_bias': dt_bias,
        'a_log': a_log,
        'd_skip': gain(ks[7], (DEPTH, SSD_HEADS)),
        'ssd_norm_g': gain(ks[8], (DEPTH, D_SSD)),
        'sgu_norm_g': gain(ks[9], (DEPTH, D_SGU)),
        'sgu_norm_b': nrm(ks[10], (DEPTH, D_SGU), 0.02),
        'w_spatial': nrm(ks[11], (DEPTH, SGU_HEADS, SGU_CHUNK, SGU_CHUNK), SGU_CHUNK ** -0.5),
        'b_spatial': gain(ks[12], (DEPTH, SGU_HEADS, SGU_CHUNK)),
        'sgu_out_g': gain(ks[13], (DEPTH, D_SGU)),
        'w_out': nrm(ks[14], (DEPTH, D_MIX, D_MODEL), D_MIX ** -0.5),
        'norm_ffn_g': gain(ks[15], (DEPTH, D_MODEL)),
        'ffn_w_gate': nrm(ks[16], (N_DENSE, D_MODEL, D_FF), D_MODEL ** -0.5),
        'ffn_w_up': nrm(ks[17], (N_DENSE, D_MODEL, D_FF), D_MODEL ** -0.5),
        'ffn_w_down': nrm(ks[18], (N_DENSE, D_FF, D_MODEL), D_FF ** -0.5),
        'moe_w_router': nrm(ks[19], (N_MOE, D_MODEL, N_EXPERTS), D_MODEL ** -0.5),
        'moe_w_gate': nrm(ks[20], (N_MOE, N_EXPERTS, D_MODEL, D_FF), D_MODEL ** -0.5),
        'moe_w_up': nrm(ks[21], (N_MOE, N_EXPERTS, D_MODEL, D_FF), D_MODEL ** -0.5),
        'moe_w_down': nrm(ks[22], (N_MOE, N_EXPERTS, D_FF, D_MODEL), D_FF ** -0.5),
        'final_norm_g': gain(ks[23], (D_MODEL,)),
    }


def reference(x, norm_mix_g, w_in, conv_w, conv_b, dt_bias, a_log, d_skip, ssd_norm_g,
              sgu_norm_g, sgu_norm_b, w_spatial, b_spatial, sgu_out_g, w_out, norm_ffn_g,
              ffn_w_gate, ffn_w_up, ffn_w_down, moe_w_router, moe_w_gate, moe_w_up,
              moe_w_down, final_norm_g):
    h = x
    for layer in range(DEPTH):
        hn = rms_norm(h, norm_mix_g[layer])
        h = h + hybrid_mixer(hn, w_in[layer], conv_w[layer], conv_b[layer], dt_bias[layer],
                             a_log[layer], d_skip[layer], ssd_norm_g[layer], sgu_norm_g[layer],
                             sgu_norm_b[layer], w_spatial[layer], b_spatial[layer],
                             sgu_out_g[layer], w_out[layer])
        hn = rms_norm(h, norm_ffn_g[layer])
        if layer % 2 == 0:
            i = layer // 2
            h = h + swiglu(hn, ffn_w_gate[i], ffn_w_up[i], ffn_w_down[i])
        else:
            i = layer // 2
            h = h + moe_swiglu(hn, moe_w_router[i], moe_w_gate[i], moe_w_up[i], moe_w_down[i])
    return rms_norm(h, final_norm_g)
```

```python
import contextlib
import numpy as np
import concourse.bass as bass
import concourse.mybir as mybir
from concourse.bass_utils import run_bass_kernel_spmd

F32 = mybir.dt.float32
BF16 = mybir.dt.bfloat16
AF = mybir.ActivationFunctionType
ALU = mybir.AluOpType

D_MODEL = 1024
D_FF = 3584
N_EXPERTS = 8
ENGS = ("tensor", "vector", "scalar", "gpsimd", "sync")


class T:
    __slots__ = ("name", "last_w", "readers")

    def __init__(self, name):
        self.name = name
        self.last_w = None
        self.readers = {}


class Op:
    __slots__ = ("eng", "fn", "deps", "flag", "val", "sem", "is_dma", "key", "inc", "blk")


class Prog:
    def __init__(self, nc, gstack, same_eng_sync=True):
        self.nc = nc
        self.gstack = gstack
        self.base_waited = {}
        self.ops = {e: [] for e in ENGS}
        self.same_eng_sync = same_eng_sync
        self.dma_cnt = {}
        self.nops = 0
        self.blk = 0
        self.fill = None
        self.last_dma = {}

    def op(self, eng, fn, reads=(), writes=(), dma_key=None, inc=16):
        o = Op()
        o.inc = inc
        o.blk = self.blk
        o.eng = eng
        o.fn = fn
        o.flag = False
        o.val = 0
        o.sem = None
        o.is_dma = dma_key is not None
        o.key = dma_key
        deps = {}
        for t in reads:
            if t.last_w is not None:
                deps[id(t.last_w)] = t.last_w
        for t in writes:
            if t.last_w is not None:
                deps[id(t.last_w)] = t.last_w
            for r in t.readers.values():
                deps[id(r)] = r
        if dma_key is not None:
            prev = self.last_dma.get(dma_key)
            if prev is not None:
                deps[id(prev)] = prev
            self.last_dma[dma_key] = o
        dl = []
        for d in deps.values():
            if d is o or d.blk != self.blk:
                continue
            if (not d.is_dma) and d.eng == eng and not o.is_dma:
                if eng == "tensor" or not self.same_eng_sync:
                    continue
            dl.append(d)
        o.deps = dl
        if eng == "tensor" and self.fill is not None and dl and not getattr(self, "_in_fill", False):
            self._in_fill = True
            n, f_out, f_l, f_r = self.fill
            for _ in range(n):
                self.op("tensor", lambda e: e.matmul(f_out, f_l, f_r, start=True, stop=True), [], [])
            self._in_fill = False
        rk = (eng, dma_key)
        for t in reads:
            t.readers[rk] = o
        for t in writes:
            t.last_w = o
            t.readers = {}
        self.ops[eng].append(o)
        self.nops += 1
        return o

    def mm(self, out, lhsT, rhs, start, stop, reads, writes):
        return self.op("tensor", lambda e: e.matmul(out, lhsT, rhs, start=start, stop=stop), reads, writes)

    def tr(self, out, in_, ident, reads, writes):
        return self.op("tensor", lambda e: e.transpose(out, in_, ident), reads, writes)

    def act(self, out, in_, func, reads, writes, bias=None, scale=None, accum_out=None, eng="scalar"):
        kw = {}
        if bias is not None:
            kw["bias"] = bias
        if scale is not None:
            kw["scale"] = scale
        if accum_out is not None:
            kw["accum_out"] = accum_out
        return self.op(eng, lambda e: e.activation(out, in_, func, **kw), reads, writes)

    def tt(self, out, in0, in1, op, reads, writes, eng="vector"):
        return self.op(eng, lambda e: e.tensor_tensor(out, in0, in1, op), reads, writes)

    def ts(self, out, in0, s1, s2, op0, op1, reads, writes, eng="vector", accum_out=None):
        if op1 is None:
            return self.op(eng, lambda e: e.tensor_scalar(out, in0, s1, None, op0), reads, writes)
        if accum_out is not None:
            return self.op(eng, lambda e: e.tensor_scalar(out, in0, s1, s2, op0, op1, accum_out=accum_out), reads, writes)
        return self.op(eng, lambda e: e.tensor_scalar(out, in0, s1, s2, op0, op1), reads, writes)

    def stt(self, out, in0, scalar, in1, op0, op1, reads, writes):
        return self.op("vector", lambda e: e.scalar_tensor_tensor(out, in0, scalar, in1, op0, op1), reads, writes)

    def copy(self, out, in_, reads, writes, eng="vector"):
        if eng == "scalar":
            return self.act(out, in_, AF.Copy, reads, writes)
        return self.op(eng, lambda e: e.tensor_copy(out, in_), reads, writes)

    def dma(self, eng, out, in_, key, reads, writes, **kw):
        return self.op(eng, lambda e: e.dma_start(out=out, in_=in_, **kw), reads, writes, dma_key=key)

    def emit(self, stack=None):
        nc = self.nc
        gs = self.gstack
        if not hasattr(self, "sems"):
            self.sems = {e: gs.enter_context(nc.semaphore("s_" + e)) for e in ENGS}
            self.keysem = {}
            self.eng_cnt = {e: 0 for e in ENGS}
        sems, keysem = self.sems, self.keysem
        for e in ENGS:
            for o in self.ops[e]:
                for d in o.deps:
                    d.flag = True
        for e in ENGS:
            c = self.eng_cnt[e]
            for o in self.ops[e]:
                if o.is_dma:
                    if o.key not in keysem:
                        keysem[o.key] = gs.enter_context(nc.semaphore("d_" + o.key))
                        self.dma_cnt[o.key] = 0
                    self.dma_cnt[o.key] += o.inc
                    o.val = self.dma_cnt[o.key]
                    o.sem = keysem[o.key]
                elif o.flag:
                    c += 1
                    o.val = c
                    o.sem = sems[e]
            self.eng_cnt[e] = c
        prog = self
        base_waited = dict(self.base_waited)

        def body(ename):
            def _f(eng):
                waited = dict(base_waited)
                for o in prog.ops[ename]:
                    need = {}
                    for d in o.deps:
                        k = id(d.sem)
                        if k not in need or need[k][1] < d.val:
                            need[k] = (d.sem, d.val)
                    for k, (s, v) in need.items():
                        if waited.get(k, 0) < v:
                            eng.wait_ge(s, v)
                            waited[k] = v
                    inst = o.fn(eng)
                    if o.is_dma:
                        if o.inc == 16:
                            inst.then_inc(o.sem, 16)
                        else:
                            inst.then_inc(o.sem)
                    elif o.flag:
                        inst.then_inc(o.sem, 1)
                if ename == "sync":
                    for k, s in keysem.items():
                        eng.wait_ge(s, prog.dma_cnt[k])
            return _f

        with nc.Block() as block:
            block.tensor(body("tensor"))
            block.vector(body("vector"))
            block.scalar(body("scalar"))
            block.gpsimd(body("gpsimd"))
            block.sync(body("sync"))
        for e in ENGS:
            self.base_waited[id(sems[e])] = self.eng_cnt[e]
        for k, sm in keysem.items():
            self.base_waited[id(sm)] = self.dma_cnt[k]
        self.ops = {e: [] for e in ENGS}
        self.blk += 1


class Ctx:
    def __init__(self, nc, stack, prog):
        self.nc = nc
        self.stack = stack
        self.P = prog
        self._n = 0

    def sb(self, name, shape, dtype):
        return self.stack.enter_context(self.nc.sbuf_tensor(name, list(shape), dtype))

    def ps(self, name, shape, dtype):
        return self.stack.enter_context(self.nc.psum_tensor(name, list(shape), dtype))

    def dram(self, name, shape, dtype, kind="Internal"):
        return self.nc.dram_tensor(name, list(shape), dtype, kind=kind).ap()


def ffn_phase(C, tag, T_tok, TH, h_src, hnT_src, gates_src, wlist, h_dst, fin_g=None,
              h_src_tiles=None, hnT_tiles=None, gates_tiles=None, h_dst_tiles=None, FC=512, emit_every=4):
    P = C.P
    NJ = TH // 128
    NTT = TH // 512
    NFS = FC // 128
    acc = C.sb(tag + "acc", [128, NJ, 1024], F32)
    hnT = C.sb(tag + "hnT", [128, 8, TH], BF16)
    wg = [C.sb(tag + "wg%d" % s, [128, 8, FC], BF16) for s in range(2)]
    wu = [C.sb(tag + "wu%d" % s, [128, 8, FC], BF16) for s in range(2)]
    wd = [C.sb(tag + "wd%d" % s, [128, NFS, 1024], BF16) for s in range(2)]
    actb = [C.sb(tag + "act%d" % s, [128, NFS, 512], BF16) for s in range(2)]
    sgb = [C.sb(tag + "sg%d" % s, [128, 512], F32) for s in range(2)]
    gts = C.sb(tag + "gts", [128, NJ, 8], F32) if gates_src is not None else None
    psum = C.psum
    t_acc = [T("acc%d" % j) for j in range(NJ)]
    t_hnT = [T("hnT%d" % j) for j in range(NTT)]
    t_wg = [T("wg%d" % s) for s in range(2)]
    t_wu = [T("wu%d" % s) for s in range(2)]
    t_wd = [T("wd%d" % s) for s in range(2)]
    t_act = [T("act%d" % s) for s in range(2)]
    t_sg = [T("sg%d" % s) for s in range(2)]
    t_gts = T("gts")
    t_ps = C.t_ps
    if fin_g is not None:
        fgrow = C.sb(tag + "fgrow", [128, 1024], F32)
        t_fg = T("fgrow")
        P.dma("sync", fgrow[:, :], fin_g.partition_broadcast(128), "ffn_fg", [], [t_fg])
        fscr = C.sb(tag + "fscr", [128, 1024], BF16)
        fss = C.sb(tag + "fss", [128, 2], F32)
        fout = [C.sb(tag + "fout%d" % s, [128, 1024], F32) for s in range(2)]
        t_fscr, t_fss = T("fscr"), T("fss")
        t_fout = [T("fout%d" % s) for s in range(2)]

    nhalf = T_tok // TH
    wci = 0
    for half in range(nhalf):
        t0 = half * TH
        for j in range(NJ):
            rd = [h_src_tiles[(t0 // 128) + j]] if h_src_tiles else []
            P.dma("sync", acc[:, j, :], h_src[t0 + j * 128: t0 + (j + 1) * 128, :], "ffn_acc%d" % j,
                  rd, [t_acc[j]])
        for tt in range(NTT):
            rd = [hnT_tiles[(t0 // 128) + tt * 4 + q] for q in range(4)] if hnT_tiles else []
            P.dma("sync", hnT[:, :, tt * 512:(tt + 1) * 512],
                  hnT_src[:, :, t0 + tt * 512: t0 + (tt + 1) * 512].rearrange("k p t -> p k t"),
                  "ffn_hnT%d" % tt, rd, [t_hnT[tt]])
        if gts is not None:
            rd = [gates_tiles[(t0 // 128) + j] for j in range(NJ)] if gates_tiles else []
            P.dma("sync", gts[:, :, :], gates_src[:, t0 // 128:(t0 + TH) // 128, :],
                  "ffn_gts", rd, [t_gts])

        pend = None

        def down(slot, tt, gi, aslot):
            for st in range(4):
                j = tt * 4 + st
                pb = 4 + 2 * (j % 2)
                for hd in range(2):
                    for fs in range(NFS):
                        P.mm(psum[:, pb + hd, :], actb[aslot][:, fs, st * 128:(st + 1) * 128],
                             wd[slot][:, fs, hd * 512:(hd + 1) * 512], fs == 0, fs == NFS - 1,
                             [t_act[aslot], t_wd[slot]], [t_ps[pb + hd]])
                po = psum[:, pb:pb + 2, :]
                av = acc[:, j, :].rearrange("p (a b) -> p a b", a=2)
                if gi is None:
                    P.tt(av, po, av, ALU.add, [t_ps[pb], t_ps[pb + 1], t_acc[j]], [t_acc[j]])
                else:
                    P.stt(av, po, gts[:, j, gi:gi + 1], av, ALU.mult, ALU.add,
                          [t_ps[pb], t_ps[pb + 1], t_acc[j], t_gts], [t_acc[j]])

        tti = 0
        for wi, (Wg, Wu, Wd, gi) in enumerate(wlist):
            if wi > 0 and emit_every and wi % emit_every == 0:
                down(*pend)
                pend = None
                P.emit()
            Fdim = Wg.shape[1]
            for fc in range(Fdim // FC):
                slot = wci % 2
                wci += 1
                P.dma("gpsimd", wg[slot][:, :, :],
                      Wg[:, fc * FC:(fc + 1) * FC].rearrange("(k p) f -> p k f", p=128),
                      "w_g%d" % slot, [], [t_wg[slot]])
                P.dma("gpsimd", wu[slot][:, :, :],
                      Wu[:, fc * FC:(fc + 1) * FC].rearrange("(k p) f -> p k f", p=128),
                      "w_u%d" % slot, [], [t_wu[slot]])
                P.dma("gpsimd", wd[slot][:, :, :],
                      Wd[fc * FC:(fc + 1) * FC, :].rearrange("(s p) d -> p s d", p=128),
                      "w_d%d" % slot, [], [t_wd[slot]])
                for tt in range(NTT):
                    aslot = tti % 2
                    tti += 1
                    for fs in range(NFS):
                        b = fs % 2
                        pg, pu = psum[:, b, :], psum[:, 2 + b, :]
                        for kc in range(8):
                            P.mm(pg, wg[slot][:, kc, fs * 128:(fs + 1) * 128], hnT[:, kc, tt * 512:(tt + 1) * 512],
                                 kc == 0, kc == 7, [t_wg[slot], t_hnT[tt]], [t_ps[b]])
                        for kc in range(8):
                            P.mm(pu, wu[slot][:, kc, fs * 128:(fs + 1) * 128], hnT[:, kc, tt * 512:(tt + 1) * 512],
                                 kc == 0, kc == 7, [t_wu[slot], t_hnT[tt]], [t_ps[2 + b]])
                        P.act(sgb[b][:, :], pg, AF.Silu, [t_ps[b]], [t_sg[b]])
                        P.tt(actb[aslot][:, fs, :], pu, sgb[b][:, :], ALU.mult,
                             [t_ps[2 + b], t_sg[b]], [t_act[aslot]])
                    if pend is not None:
                        down(*pend)
                    pend = (slot, tt, gi, aslot)
        down(*pend)
        pend = None
        for j in range(NJ):
            jj = (t0 // 128) + j
            if fin_g is None:
                wr = [h_dst_tiles[jj]] if h_dst_tiles else []
                P.dma("sync", h_dst[t0 + j * 128: t0 + (j + 1) * 128, :], acc[:, j, :], "ffn_st%d" % (j % 2),
                      [t_acc[j]], wr)
            else:
                s = j % 2
                P.act(fscr[:, :], acc[:, j, :], AF.Square, [t_acc[j]], [t_fscr, t_fss], accum_out=fss[:, 0:1])
                rstd_ops(P, fss[:, 1:2], fss[:, 0:1], 1.0 / 1024, [t_fss], [t_fss])
                P.stt(fout[s][:, :], acc[:, j, :], fss[:, 1:2], fgrow[:, :], ALU.mult, ALU.mult,
                      [t_acc[j], t_fss, t_fg], [t_fout[s]])
                P.dma("sync", h_dst[t0 + j * 128: t0 + (j + 1) * 128, :], fout[s][:, :], "ffn_st%d" % s,
                      [t_fout[s]], [])


def rstd_ops(P, out, ss, inv_n, reads, writes, eps=1e-6):
    P.ts(out, ss, inv_n, eps, ALU.mult, ALU.add, reads, writes)
    P.act(out, out, AF.Sqrt, reads, writes)
    P.op("vector", lambda e: e.reciprocal(out, out), reads, writes)


def host_consts():
    import ml_dtypes
    i = np.arange(128)
    ident = np.eye(128, dtype=np.float32)
    triU = (i[:, None] <= i[None, :]).astype(np.float32)
    ones = np.ones((128, 128), np.float32)
    cf = np.concatenate([ident, triU, ones], axis=1)
    sc = [(i[:, None] == (i[None, :] - (3 - k))).astype(np.float32) for k in range(3)]
    sp = [(i[:, None] == (128 + i[None, :] - (3 - k))).astype(np.float32) for k in range(3)]
    mneg = np.where(i[None, :] < i[:, None], -30000.0, 0.0).astype(np.float32)
    mneg4 = np.tile(mneg, (1, 4))
    onesrow = np.zeros((128, 128), np.float32)
    onesrow[0, :] = 1.0
    cb = np.concatenate([ident] + sc + sp + [mneg4, onesrow, ones, triU], axis=1).astype(ml_dtypes.bfloat16)
    return cf, cb


class Consts:
    pass


def load_consts(C, cf_ap, cb_ap):
    P = C.P
    K = Consts()
    gs = C.P.gstack
    cf = gs.enter_context(C.nc.sbuf_tensor("cst_f", [128, 384], F32))
    cb = gs.enter_context(C.nc.sbuf_tensor("cst_b", [128, 1792], BF16))
    K.t = T("consts")
    P.dma("sync", cf[:, :], cf_ap, "const", [], [K.t])
    P.dma("sync", cb[:, :], cb_ap, "const", [], [K.t])
    K.identF = cf[:, 0:128]
    K.triU = cf[:, 128:256]
    K.onesF = cf[:, 256:384]
    K.identB = cb[:, 0:128]
    K.Sc = [cb[:, 128 * (1 + k):128 * (2 + k)] for k in range(3)]
    K.Sp = [cb[:, 128 * (4 + k):128 * (5 + k)] for k in range(3)]
    K.mneg = cb[:, 896:1408]
    K.onesrow = cb[0:1, 1408:1536]
    K.onesB = cb[:, 1536:1664]
    K.triUB = cb[:, 1664:1792]
    return K


def rstd_from_ss(P, st, i_ss, i_out, inv_n, tl, eps=1e-6):
    rstd_ops(P, st[:, i_out:i_out + 1], st[:, i_ss:i_ss + 1], inv_n, [tl], [tl], eps)


def mixer_A(C, K, L, T_tok, h_src, w_in, norm_g, conv_w, conv_b, dt_bias, a_log, d_skip, ssd_g,
            state_only, st_in, st_out, flag_ap, hnT_dst, yssd_dst):
    P = C.P
    NCH = T_tok // 128
    tg = "A%d%s_" % (L, "p" if state_only else "m")
    sb = C.sb
    NW = 2576
    win = sb(tg + "win", [128, 8, NW], BF16)
    gmix = sb(tg + "gmix", [128, 1024], F32)
    gssd = sb(tg + "gssd", [128, 1024], F32)
    cw = sb(tg + "cw", [128, 4, 1536], BF16)
    cbias = sb(tg + "cbias", [1, 1536], BF16)
    sm = sb(tg + "sm", [128, 64], F32)
    t_win, t_row, t_sm = T("win"), T("rows"), T("sm")
    blocks = [(0, 512), (512, 512), (1024, 512), (1536, 512), (2048, 512), (2560, 16)]
    for (c0, cn) in blocks:
        P.dma("gpsimd", win[:, :, c0:c0 + cn], w_in[:, c0:c0 + cn].rearrange("(k p) f -> p k f", p=128),
              "cw_a_w", [], [t_win])
    P.dma("sync", gmix[:, :], norm_g.partition_broadcast(128), "cr_a", [], [t_row])
    P.dma("sync", gssd[:, :], ssd_g.partition_broadcast(128), "cr_a", [], [t_row])
    for k in range(4):
        P.dma("gpsimd", cw[:, k, :], conv_w[k, :].partition_broadcast(128), "cw_a", [], [t_row])
    P.dma("gpsimd", cbias[:, :], conv_b.unsqueeze(0), "cw_a", [], [t_row])
    P.dma("sync", sm[:, 0:16], dt_bias.partition_broadcast(128), "cr_a", [], [t_sm])
    P.dma("sync", sm[:, 16:32], a_log.partition_broadcast(128), "cr_a", [], [t_sm])
    P.dma("sync", sm[:, 32:48], d_skip.partition_broadcast(128), "cr_a", [], [t_sm])
    P.dma("sync", sm[:, 48:49], flag_ap, "cr_a", [], [t_sm])
    P.act(sm[:, 16:32], sm[:, 16:32], AF.Exp, [t_sm], [t_sm])
    P.ts(sm[:, 16:32], sm[:, 16:32], -1.0, None, ALU.mult, None, [t_sm], [t_sm])
    dtb, arow, dsk, flg = sm[:, 0:16], sm[:, 16:32], sm[:, 32:48], sm[:, 48:49]

    DEP = 3 if state_only else 2

    def two(name, shape, dt):
        return [sb(tg + name + "%d" % s, shape, dt) for s in range(DEP)]

    def twoT(name):
        return [T(name + "%d" % s) for s in range(DEP)]

    hin = two("hin", [128, 1024], F32)
    scr_a = sb(tg + "scra", [128, 1024], BF16)
    st_a = sb(tg + "sta", [128, 4], F32)
    hn = sb(tg + "hn", [128, 1024], F32)
    hnT = two("hnT", [128, 8, 128], BF16)
    raw = sb(tg + "raw", [128, 1536], BF16)
    rw = two("rw", [128, 4, 1536], BF16)
    xa = two("xa", [128, 1536], BF16)
    dts = two("dts", [128, 128], F32)
    xdtd = two("xdtd", [128, 1024], BF16)
    S = sb(tg + "S", [128, 1024], F32)
    t_hin, t_hnT, t_rw, t_xa, t_dts, t_xdtd = [twoT(n) for n in ("hin", "hnT", "rw", "xa", "dts", "xdtd")]
    t_scra, t_sta, t_hn, t_raw, t_S = [T(n) for n in ("scra", "sta", "hn", "raw", "S")]
    t_ps = C.t_ps
    psum = C.psum
    if not state_only:
        scr_b = sb(tg + "scrb", [128, 1024], BF16)
        st_b = sb(tg + "stb", [128, 4], F32)
        sz = two("sz", [128, 1024], BF16)
        ncs = two("ncs", [128, 32], F32)
        rhs1 = sb(tg + "rhs1", [128, 2, 16, 128], BF16)
        dab = sb(tg + "dab", [128, 32], BF16)
        bct = two("bct", [128, 4, 128], BF16)
        cbT = two("cbT", [128, 2, 128], BF16)
        decT = two("decT", [128, 16, 128], BF16)
        scT = two("scT", [128, 16, 128], BF16)
        xdt = two("xdt", [128, 1024], BF16)
        xds = two("xds", [128, 1024], BF16)
        Sb = two("Sb", [128, 1024], BF16)
        y1 = sb(tg + "y1", [128, 1024], F32)
        y2 = sb(tg + "y2", [128, 1024], F32)
        yn = two("yn", [128, 1024], BF16)
        t_sz, t_ncs, t_bct, t_cbT, t_decT, t_scT, t_xdt, t_xds, t_Sb, t_yn = [twoT(n) for n in (
            "sz", "ncs", "bct", "cbT", "decT", "scT", "xdt", "xds", "Sb", "yn")]
        t_scrb, t_stb, t_rhs1, t_y1, t_y2 = [T(n) for n in ("scrb", "stb", "rhs1", "y1", "y2")]

    if state_only:
        P.op("gpsimd", lambda e: e.memset(S[:, :], 0.0), [], [t_S])
        P.op("gpsimd", lambda e: e.memset(rw[DEP - 1][:, :, :], 0.0), [], [t_rw[DEP - 1]])
    else:
        P.dma("sync", S[:, :], st_in[0:128, 0:1024], "st_ld", [], [t_S])
        P.ts(S[:, :], S[:, :], flg, None, ALU.mult, None, [t_S, t_sm], [t_S])
        P.copy(Sb[0][:, :], S[:, :], [t_S], [t_Sb[0]], eng="scalar")
        rawh = y1
        P.op("gpsimd", lambda e: e.memset(rawh[:, :], 0.0), [], [t_y1])
        P.dma("sync", rawh[125:128, 0:768], st_in[128:131, 0:768], "st_ld2", [t_y1], [t_y1])
        P.ts(rawh[:, 0:768], rawh[:, 0:768], flg, None, ALU.mult, None, [t_y1, t_sm], [t_y1])
        for k in range(3):
            P.tt(rw[DEP - 1][:, k, 0:768], rawh[:, 0:768], cw[:, k, 0:768], ALU.mult, [t_y1, t_row], [t_rw[DEP - 1]],
                 eng="gpsimd")
        P.op("gpsimd", lambda e: e.memset(rawh[:, :], 0.0), [t_y1], [t_y1]) if False else None
        P.dma("sync", rawh[125:128, 0:768], st_in[128:131, 768:1536], "st_ld2", [t_y1, t_rw[DEP - 1]], [t_y1])
        P.ts(rawh[:, 0:768], rawh[:, 0:768], flg, None, ALU.mult, None, [t_y1, t_sm], [t_y1])
        for k in range(3):
            P.tt(rw[DEP - 1][:, k, 768:1536], rawh[:, 0:768], cw[:, k, 768:1536], ALU.mult, [t_y1, t_row],
                 [t_rw[DEP - 1]], eng="gpsimd")

    ps_hn = psum[:, 0:2, :].rearrange("p a (k t) -> p (a k) t", k=4)
    h16 = lambda ap: ap.rearrange("p (h d) -> p h d", h=16)
    pbs = [2, 3]

    def chunk(c):
        s2 = c % DEP
        sp = (c - 1) % DEP
        sn = (c + 1) % DEP
        tok = slice(c * 128, (c + 1) * 128)
        d_ = dts[s2]
        td = t_dts[s2]
        for cc in ([0, 1] if c == 0 else [c + 1]):
            if cc < NCH:
                P.dma("sync", hin[cc % DEP][:, :], h_src[cc * 128:(cc + 1) * 128, :], "a_h%d" % (cc % DEP), [],
                      [t_hin[cc % DEP]])
        P.act(scr_a[:, :], hin[s2][:, :], AF.Square, [t_hin[s2]], [t_scra, t_sta], accum_out=st_a[:, 0:1])
        rstd_from_ss(P, st_a, 0, 1, 1.0 / 1024, t_sta)
        P.stt(hn[:, :], hin[s2][:, :], st_a[:, 1:2], gmix[:, :], ALU.mult, ALU.mult,
              [t_hin[s2], t_sta, t_row], [t_hn])
        yield
        for k in range(8):
            P.tr(ps_hn[:, k, :], hn[:, k * 128:(k + 1) * 128], K.identF, [t_hn, K.t], [t_ps[k // 4]])
        P.copy(hnT[s2][:, :, :], ps_hn, [t_ps[0], t_ps[1]], [t_hnT[s2]], eng="scalar")
        if not state_only:
            P.dma("scalar", hnT_dst[:, :, tok].rearrange("k p t -> p k t"), hnT[s2][:, :, :], "a_hnT%d" % s2,
                  [t_hnT[s2]], [])
        bi = 0
        for (c0, cn) in blocks:
            if c0 >= 1024 or state_only:
                continue
            pb = pbs[bi % 2]
            bi += 1
            for k in range(8):
                P.mm(psum[:, pb, 0:cn], hnT[s2][:, k, :], win[:, k, c0:c0 + cn], k == 0, k == 7,
                     [t_hnT[s2], t_win], [t_ps[pb]])
            P.act(sz[s2][:, c0:c0 + cn], psum[:, pb, 0:cn], AF.Silu, [t_ps[pb]], [t_sz[s2]])
        yield
        lite = state_only and c != NCH - 1
        for (c0, cn) in blocks:
            if c0 < 1024:
                continue
            if lite and c0 == 2048:
                cn = 256
            pb = pbs[bi % 2]
            bi += 1
            for k in range(8):
                P.mm(psum[:, pb, 0:cn], hnT[s2][:, k, :], win[:, k, c0:c0 + cn], k == 0, k == 7,
                     [t_hnT[s2], t_win], [t_ps[pb]])
            if c0 < 2560:
                P.copy(raw[:, c0 - 1024:c0 - 1024 + cn], psum[:, pb, 0:cn], [t_ps[pb]], [t_raw], eng="scalar")
            else:
                P.tt(d_[:, 0:16], psum[:, pb, 0:16], dtb, ALU.add, [t_ps[pb], t_sm], [td])
        P.act(d_[:, 16:32], d_[:, 0:16], AF.Exp, [td], [td])
        P.act(d_[:, 32:48], d_[:, 16:32], AF.Ln, [td], [td], bias=1.0)
        P.tt(d_[:, 48:64], d_[:, 32:48], arow, ALU.mult, [td, t_sm], [td])
        ncv = 1280 if lite else 1536
        for k in range(4):
            P.tt(rw[s2][:, k, 0:ncv], raw[:, 0:ncv], cw[:, k, 0:ncv], ALU.mult, [t_raw, t_row], [t_rw[s2]])
        yield
        for b3 in range(3):
            pb = pbs[b3 % 2]
            wd_ = 256 if (state_only and b3 == 2) else 512
            cs_ = slice(b3 * 512, b3 * 512 + wd_)
            po_ = psum[:, pb, 0:wd_]
            P.mm(po_, K.onesrow, cbias[0:1, cs_], True, False, [K.t, t_row], [t_ps[pb]])
            for k in range(3):
                P.mm(po_, K.Sp[k], rw[sp][:, k, cs_], False, False, [K.t, t_rw[sp]], [t_ps[pb]])
            for k in range(3):
                P.mm(po_, K.Sc[k], rw[s2][:, k, cs_], False, False, [K.t, t_rw[s2]], [t_ps[pb]])
            P.mm(po_, K.identB, rw[s2][:, 3, cs_], False, True, [K.t, t_rw[s2]], [t_ps[pb]])
            P.act(xa[s2][:, cs_], po_, AF.Silu, [t_ps[pb]], [t_xa[s2]])
        P.mm(psum[:, 4, 0:16], K.triU, d_[:, 48:64], True, True, [K.t, td], [t_ps[4]])
        P.mm(psum[:, 4, 16:32], K.onesF, d_[:, 48:64], True, True, [K.t, td], [t_ps[4]])
        P.copy(d_[:, 64:96], psum[:, 4, 0:32], [t_ps[4]], [td])
        P.tt(d_[:, 96:112], d_[:, 80:96], d_[:, 64:80], ALU.subtract, [td], [td])
        P.act(d_[:, 96:112], d_[:, 96:112], AF.Exp, [td], [td])
        P.act(d_[:, 112:128], d_[:, 80:96], AF.Exp, [td], [td])
        yield
        x3 = h16(xa[s2][:, 0:1024])
        dt_b = d_[:, 32:48].unsqueeze(2).to_broadcast([128, 16, 64])
        dte_b = d_[:, 96:112].unsqueeze(2).to_broadcast([128, 16, 64])
        ebl_b = d_[:, 112:128].unsqueeze(2).to_broadcast([128, 16, 64])
        if state_only:
            P.tt(h16(xdtd[s2][:, :]), x3, dt_b, ALU.mult, [t_xa[s2], td], [t_xdtd[s2]], eng="gpsimd")
            P.tt(h16(xdtd[s2][:, :]), h16(xdtd[s2][:, :]), dte_b, ALU.mult, [t_xdtd[s2], td], [t_xdtd[s2]], eng="gpsimd")
        else:
            P.tt(h16(xdt[s2][:, :]), x3, dt_b, ALU.mult, [t_xa[s2], td], [t_xdt[s2]], eng="gpsimd")
            P.tt(h16(xdtd[s2][:, :]), h16(xdt[s2][:, :]), dte_b, ALU.mult, [t_xdt[s2], td], [t_xdtd[s2]], eng="gpsimd")
            P.tt(h16(xds[s2][:, :]), x3, dsk.unsqueeze(2).to_broadcast([128, 16, 64]), ALU.mult,
                 [t_xa[s2], t_sm], [t_xds[s2]], eng="gpsimd")
            n_ = ncs[s2]
            P.ts(n_[:, 0:16], d_[:, 64:80], -1.0, None, ALU.mult, None, [td], [t_ncs[s2]])
            P.act(n_[:, 16:32], d_[:, 64:80], AF.Exp, [td], [t_ncs[s2]])
            ps_t = psum[:, 7, 256:512].bitcast(BF16).rearrange("p (a t) -> p a t", a=4)
            for q in range(4):
                P.tr(ps_t[:, q, :], xa[s2][:, 1024 + q * 128:1024 + (q + 1) * 128], K.identB, [t_xa[s2], K.t], [t_ps[7]])
            P.copy(bct[s2][:, :, :], ps_t, [t_ps[7]], [t_bct[s2]])
            for g in range(2):
                P.mm(psum[:, 7, g * 128:(g + 1) * 128], bct[s2][:, g, :], bct[s2][:, 2 + g, :], True, True,
                     [t_bct[s2]], [t_ps[7]])
            P.copy(cbT[s2][:, :, :], psum[:, 7, 0:256].rearrange("p (g l) -> p g l", g=2), [t_ps[7]], [t_cbT[s2]],
                   eng="scalar")
            P.copy(dab[:, 0:16], d_[:, 48:64], [td], [t_rhs1])
            P.tt(dab[:, 16:32], d_[:, 48:64], dab[:, 0:16], ALU.subtract, [td, t_rhs1], [t_rhs1])
            for hl in range(2):
                P.tt(rhs1[:, hl, :, :], K.triUB.unsqueeze(1).to_broadcast([128, 16, 128]),
                     dab[:, hl * 16:(hl + 1) * 16].unsqueeze(2).to_broadcast([128, 16, 128]), ALU.mult,
                     [K.t, t_rhs1], [t_rhs1])
            yield
            for hb in range(2):
                for q in range(2):
                    h0 = hb * 8 + q * 4
                    P.mm(psum[:, 5 + q, :], K.onesB, rhs1[:, 0, h0:h0 + 4, :].rearrange("p h l -> p (h l)"), True, False,
                         [K.t, t_rhs1], [t_ps[5 + q]])
                    P.mm(psum[:, 5 + q, :], K.onesB, rhs1[:, 1, h0:h0 + 4, :].rearrange("p h l -> p (h l)"), False, False,
                         [K.t, t_rhs1], [t_ps[5 + q]])
                    P.mm(psum[:, 5 + q, :], K.identB, K.mneg, False, True, [K.t], [t_ps[5 + q]])
                    for hh in range(4):
                        h = h0 + hh
                        P.act(decT[s2][:, h, :], psum[:, 5 + q, hh * 128:(hh + 1) * 128], AF.Exp,
                              [t_ps[5 + q], t_ncs[s2]], [t_decT[s2]], bias=n_[:, h:h + 1])
                P.tt(scT[s2][:, hb * 8:(hb + 1) * 8, :], decT[s2][:, hb * 8:(hb + 1) * 8, :],
                     cbT[s2][:, hb, :].unsqueeze(1).to_broadcast([128, 8, 128]), ALU.mult,
                     [t_decT[s2], t_cbT[s2]], [t_scT[s2]])
                yield
        S3 = h16(S[:, :])
        if state_only:
            for g in range(2):
                P.mm(psum[:, 5 + g, :], xa[s2][:, 1024 + g * 128:1024 + (g + 1) * 128], xdtd[s2][:, g * 512:(g + 1) * 512],
                     True, True, [t_xa[s2], t_xdtd[s2]], [t_ps[5 + g]])
            P.tt(S3, S3, ebl_b, ALU.mult, [t_S, td], [t_S])
            P.tt(S[:, :].rearrange("p (a b) -> p a b", a=2), S[:, :].rearrange("p (a b) -> p a b", a=2), psum[:, 5:7, :],
                 ALU.add, [t_S, t_ps[5], t_ps[6]], [t_S])
            return
        e_b = ncs[s2][:, 16:32].unsqueeze(2).to_broadcast([128, 16, 64])
        for g in range(2):
            gs_ = slice(g * 512, (g + 1) * 512)
            P.mm(psum[:, 5, :], K.identB, xds[s2][:, gs_], True, False, [K.t, t_xds[s2]], [t_ps[5]])
            for hh in range(8):
                h = g * 8 + hh
                P.mm(psum[:, 5, hh * 64:(hh + 1) * 64], scT[s2][:, h, :], xdt[s2][:, h * 64:(h + 1) * 64], False, hh == 7,
                     [t_scT[s2], t_xdt[s2]], [t_ps[5]])
            P.mm(psum[:, 6, :], bct[s2][:, 2 + g, :], Sb[s2][:, gs_], True, True, [t_bct[s2], t_Sb[s2]], [t_ps[6]])
            P.mm(psum[:, 7, :], xa[s2][:, 1024 + g * 128:1024 + (g + 1) * 128], xdtd[s2][:, gs_], True, True,
                 [t_xa[s2], t_xdtd[s2]], [t_ps[7]])
            Sg = S[:, gs_].rearrange("p (h d) -> p h d", h=8)
            P.tt(Sg, Sg, d_[:, 112 + g * 8:120 + g * 8].unsqueeze(2).to_broadcast([128, 8, 64]), ALU.mult, [t_S, td], [t_S])
            P.tt(S[:, gs_], S[:, gs_], psum[:, 7, :], ALU.add, [t_S, t_ps[7]], [t_S])
            P.copy(Sb[sn][:, gs_], S[:, gs_], [t_S], [t_Sb[sn]], eng="scalar")
            P.tt(y1[:, gs_].rearrange("p (h d) -> p h d", h=8), psum[:, 6, :].rearrange("p (h d) -> p h d", h=8),
                 ncs[s2][:, 16 + g * 8:24 + g * 8].unsqueeze(2).to_broadcast([128, 8, 64]), ALU.mult,
                 [t_ps[6], t_ncs[s2]], [t_y1])
            P.tt(y1[:, gs_], psum[:, 5, :], y1[:, gs_], ALU.add, [t_ps[5], t_y1], [t_y1])
        P.tt(y2[:, :], y1[:, :], sz[s2][:, :], ALU.mult, [t_y1, t_sz[s2]], [t_y2])
        P.act(scr_b[:, :], y2[:, :], AF.Square, [t_y2], [t_scrb, t_stb], accum_out=st_b[:, 0:1])
        rstd_from_ss(P, st_b, 0, 1, 1.0 / 1024, t_stb)
        P.stt(yn[s2][:, :], y2[:, :], st_b[:, 1:2], gssd[:, :], ALU.mult, ALU.mult, [t_y2, t_stb, t_row], [t_yn[s2]])
        P.dma("sync", yssd_dst[tok, :], yn[s2][:, :], "a_y%d" % s2, [t_yn[s2]], [])

    if NFILL > 0:
        P.fill = (NFILL, psum[:, 4, 64:512], K.identB, K.mneg[:, 0:448])
    run_pipeline(chunk, NCH, 2 if state_only else 4, DEP)
    P.fill = None
    if state_only:
        P.dma("sync", st_out[0:128, 0:1024], S[:, :], "st_st", [t_S], [])
        P.dma("gpsimd", st_out[128:131, :], raw[125:128, :], "st_st2", [t_raw], [])


def run_pipeline(body, n, offset, depth=2):
    active = []
    nxt = 0
    while nxt < n or active:
        if nxt < n and (not active or (len(active) < depth and active[-1][1] >= offset)):
            active.append([body(nxt), 0])
            nxt += 1
        for a in list(active):
            try:
                next(a[0])
                a[1] += 1
            except StopIteration:
                active.remove(a)


def mixer_B(C, K, L, T_tok, h_src, h_dst, hnT_src, yssd_src, w_in, sgu_g, sgu_b, wsT_ap, bspT_ap, sguo_g,
            w_out, ffn_g, hnT2_dst, w_router, gates_dst):
    P = C.P
    NCH = T_tok // 128
    tg = "B%d_" % L
    sb = C.sb
    moe = w_router is not None
    win = sb(tg + "win", [128, 8, 2048], BF16)
    wout = sb(tg + "wout", [128, 16, 1024], BF16)
    rows = sb(tg + "rows", [128, 4, 1024], F32)
    wsf = sb(tg + "wsf", [128, 8, 128], F32)
    wsT = sb(tg + "wsT", [128, 8, 128], BF16)
    bsp = sb(tg + "bsp", [128, 8], F32)
    t_win, t_wout, t_rows, t_ws = T("win"), T("wout"), T("rows"), T("ws")
    for q in range(4):
        P.dma("gpsimd", win[:, :, q * 512:(q + 1) * 512],
              w_in[:, 2576 + q * 512:2576 + (q + 1) * 512].rearrange("(k p) f -> p k f", p=128), "cw_b_w", [], [t_win])
    for q in range(2):
        P.dma("gpsimd", wout[:, :, q * 512:(q + 1) * 512],
              w_out[:, q * 512:(q + 1) * 512].rearrange("(k p) f -> p k f", p=128), "cw_b_o", [], [t_wout])
    for i, r in enumerate((sgu_g, sgu_b, sguo_g, ffn_g)):
        P.dma("sync", rows[:, i, :], r.partition_broadcast(128), "cr_b_r", [], [t_rows])
    P.dma("sync", wsf[:, :, :], wsT_ap, "cr_b", [], [t_ws])
    P.dma("sync", bsp[:, :], bspT_ap, "cr_b", [], [t_ws])
    P.tt(wsT[:, :, :], wsf[:, :, :], K.triU.unsqueeze(1).to_broadcast([128, 8, 128]), ALU.mult, [t_ws, K.t], [t_ws])
    if moe:
        wr = sb(tg + "wr", [128, 8, 8], F32)
        wrh = sb(tg + "wrh", [128, 8, 8], BF16)
        wrl = sb(tg + "wrl", [128, 8, 8], BF16)
        t_wr = T("wr")
        P.dma("sync", wr[:, :, :], w_router, "cr_b2", [], [t_wr])
        P.copy(wrh[:, :, :], wr[:, :, :], [t_wr], [t_wr])
        P.tt(wrl[:, :, :], wr[:, :, :], wrh[:, :, :], ALU.subtract, [t_wr], [t_wr])
    gsgu, bsgu, gsguo, gffn = rows[:, 0, :], rows[:, 1, :], rows[:, 2, :], rows[:, 3, :]

    DEP = 3

    def two(name, shape, dt):
        return [sb(tg + name + "%d" % s, shape, dt) for s in range(DEP)]

    def twoT(name):
        return [T(name + "%d" % s) for s in range(DEP)]

    hin = two("hin", [128, 1024], F32)
    hnT = two("hnT", [128, 8, 128], BF16)
    ycat = two("ycat", [128, 2048], BF16)
    u = two("u", [128, 1024], BF16)
    v = sb(tg + "v", [128, 1024], F32)
    vn = sb(tg + "vn", [128, 1024], F32)
    vnb = sb(tg + "vnb", [128, 1024], BF16)
    scr_a = sb(tg + "scra", [128, 1024], BF16)
    scr_b = sb(tg + "scrb", [128, 1024], BF16)
    st_l = sb(tg + "stl", [128, 16], F32)
    st_a = sb(tg + "sta", [128, 4], F32)
    st_b = sb(tg + "stb", [128, 4], F32)
    t1 = sb(tg + "t1", [128, 1024], F32)
    ycT = sb(tg + "ycT", [128, 16, 128], BF16)
    hnew = two("hnew", [128, 1024], F32)
    hn2 = sb(tg + "hn2", [128, 1024], F32)
    hn2T = two("hn2T", [128, 8, 128], BF16)
    t_hin, t_hnT, t_ycat, t_u, t_hnew, t_hn2T = [twoT(n) for n in ("hin", "hnT", "ycat", "u", "hnew", "hn2T")]
    t_v, t_vn, t_vnb, t_scra, t_scrb, t_stl, t_sta, t_stb, t_t1, t_ycT, t_hn2 = [T(n) for n in (
        "v", "vn", "vnb", "scra", "scrb", "stl", "sta", "stb", "t1", "ycT", "hn2")]
    if moe:
        hn2Tl = sb(tg + "hn2Tl", [128, 8, 128], BF16)
        lg = two("lg", [128, 48], F32)
        t_hn2Tf = T("hn2Tl")
        t_lg = twoT("lg")
        gall = sb(tg + "gall", [128, NCH, 8], F32)
        t_gall = T("gall")
    psum, t_ps = C.psum, C.t_ps
    ps_tr = psum[:, 0:2, :].rearrange("p a (k t) -> p (a k) t", k=4)

    def chunk(c):
        s2 = c % DEP
        tok = slice(c * 128, (c + 1) * 128)
        for cc in ([0, 1] if c == 0 else [c + 1]):
            if cc < NCH:
                sc_ = cc % DEP
                tk = slice(cc * 128, (cc + 1) * 128)
                P.dma("sync", hnT[sc_][:, :, :], hnT_src[:, :, tk].rearrange("k p t -> p k t"), "b_hnT%d" % sc_, [],
                      [t_hnT[sc_]])
                P.dma("sync", ycat[sc_][:, 0:1024], yssd_src[tk, :], "b_y%d" % sc_, [], [t_ycat[sc_]])
                P.dma("sync", hin[sc_][:, :], h_src[tk, :], "b_h%d" % sc_, [], [t_hin[sc_]])
        for q in range(4):
            pb = q % 2
            for k in range(8):
                P.mm(psum[:, pb, :], hnT[s2][:, k, :], win[:, k, q * 512:(q + 1) * 512], k == 0, k == 7,
                     [t_hnT[s2], t_win], [t_ps[pb]])
            if q < 2:
                P.act(u[s2][:, q * 512:(q + 1) * 512], psum[:, pb, :], AF.Gelu, [t_ps[pb]], [t_u[s2]])
            else:
                P.act(v[:, (q - 2) * 512:(q - 1) * 512], psum[:, pb, :], AF.Gelu, [t_ps[pb]], [t_v])
        yield
        for q in range(2):
            P.op("vector", lambda e, q=q: e.bn_stats(st_l[:, q * 6:6 + q * 6], v[:, q * 512:(q + 1) * 512]),
                 [t_v], [t_stl])
        P.op("vector", lambda e: e.bn_aggr(st_l[:, 12:14], st_l[:, 0:12]), [t_stl], [t_stl])
        rstd_ops(P, st_l[:, 14:15], st_l[:, 13:14], 1.0, [t_stl], [t_stl])
        P.ts(vn[:, :], v[:, :], st_l[:, 12:13], st_l[:, 14:15], ALU.subtract, ALU.mult, [t_v, t_stl], [t_vn])
        P.tt(vn[:, :], vn[:, :], gsgu, ALU.mult, [t_vn, t_rows], [t_vn], eng="gpsimd")
        P.tt(vnb[:, :], vn[:, :], bsgu, ALU.add, [t_vn, t_rows], [t_vnb], eng="gpsimd")
        yield
        for h in range(8):
            pb = 6 + h // 4
            P.mm(psum[:, pb, (h % 4) * 128:(h % 4 + 1) * 128], wsT[:, h, :], vnb[:, h * 128:(h + 1) * 128], True, True,
                 [t_ws, t_vnb], [t_ps[pb]])
        P.tt(t1[:, :].rearrange("p (h d) -> p h d", h=8), psum[:, 6:8, :].rearrange("p a (h d) -> p (a h) d", h=4),
             bsp[:, :].unsqueeze(2).to_broadcast([128, 8, 128]), ALU.add, [t_ps[6], t_ps[7], t_ws], [t_t1])
        P.tt(t1[:, :], t1[:, :], u[s2][:, :], ALU.mult, [t_t1, t_u[s2]], [t_t1])
        P.act(scr_a[:, :], t1[:, :], AF.Square, [t_t1], [t_scra, t_sta], accum_out=st_a[:, 0:1])
        rstd_from_ss(P, st_a, 0, 1, 1.0 / 1024, t_sta)
        P.stt(ycat[s2][:, 1024:2048], t1[:, :], st_a[:, 1:2], gsguo, ALU.mult, ALU.mult, [t_t1, t_sta, t_rows],
              [t_ycat[s2]])
        yield
        ps_y = psum[:, 2:4, :].rearrange("p a b -> p (a b)").bitcast(BF16).rearrange("p (k t) -> p k t", k=16)
        for k in range(16):
            P.tr(ps_y[:, k, :], ycat[s2][:, k * 128:(k + 1) * 128], K.identB, [t_ycat[s2], K.t], [t_ps[2 + k // 8]])
        P.copy(ycT[:, :, :], ps_y, [t_ps[2], t_ps[3]], [t_ycT], eng="scalar")
        yield
        for hd in range(2):
            for k in range(16):
                P.mm(psum[:, 4 + hd, :], ycT[:, k, :], wout[:, k, hd * 512:(hd + 1) * 512], k == 0, k == 15,
                     [t_ycT, t_wout], [t_ps[4 + hd]])
        P.tt(hnew[s2][:, :].rearrange("p (a b) -> p a b", a=2), psum[:, 4:6, :],
             hin[s2][:, :].rearrange("p (a b) -> p a b", a=2), ALU.add, [t_ps[4], t_ps[5], t_hin[s2]], [t_hnew[s2]])
        P.dma("sync", h_dst[tok, :], hnew[s2][:, :], "b_ho%d" % s2, [t_hnew[s2]], [])
        yield
        P.act(scr_b[:, :], hnew[s2][:, :], AF.Square, [t_hnew[s2]], [t_scrb, t_stb], accum_out=st_b[:, 0:1])
        rstd_from_ss(P, st_b, 0, 1, 1.0 / 1024, t_stb)
        P.stt(hn2[:, :], hnew[s2][:, :], st_b[:, 1:2], gffn, ALU.mult, ALU.mult, [t_hnew[s2], t_stb, t_rows], [t_hn2])
        for k in range(8):
            P.tr(ps_tr[:, k, :], hn2[:, k * 128:(k + 1) * 128], K.identF, [t_hn2, K.t], [t_ps[k // 4]])
        P.copy(hn2T[s2][:, :, :], ps_tr, [t_ps[0], t_ps[1]], [t_hn2T[s2]], eng="scalar")
        P.dma("scalar", hnT2_dst[:, :, tok].rearrange("k p t -> p k t"), hn2T[s2][:, :, :], "b_h2T%d" % s2,
              [t_hn2T[s2]], [])
        if moe:
            l_ = lg[s2]
            tl = t_lg[s2]
            P.tt(hn2Tl[:, :, :], ps_tr, hn2T[s2][:, :, :], ALU.subtract, [t_ps[0], t_ps[1], t_hn2T[s2]], [t_hn2Tf])
            for k in range(8):
                P.mm(psum[:, 4, 0:8], hn2T[s2][:, k, :], wrh[:, k, :], k == 0, False, [t_hn2T[s2], t_wr], [t_ps[4]])
                P.mm(psum[:, 4, 0:8], hn2T[s2][:, k, :], wrl[:, k, :], False, False, [t_hn2T[s2], t_wr], [t_ps[4]])
                P.mm(psum[:, 4, 0:8], hn2Tl[:, k, :], wrh[:, k, :], False, k == 7, [t_hn2Tf, t_wr], [t_ps[4]])
            P.copy(l_[:, 0:8], psum[:, 4, 0:8], [t_ps[4]], [tl])
            P.op("vector", lambda e, l_=l_: e.max(l_[:, 8:16], l_[:, 0:8]), [tl], [tl])
            P.ts(l_[:, 16:24], l_[:, 0:8], l_[:, 9:10], None, ALU.is_ge, None, [tl], [tl])
            P.ts(l_[:, 40:41], l_[:, 8:9], -1.0, None, ALU.mult, None, [tl], [tl])
            P.act(l_[:, 24:32], l_[:, 0:8], AF.Exp, [tl], [tl], bias=l_[:, 40:41])
            P.tt(l_[:, 24:32], l_[:, 24:32], l_[:, 16:24], ALU.mult, [tl], [tl])
            P.op("vector", lambda e, l_=l_: e.tensor_reduce(l_[:, 41:42], l_[:, 24:32], mybir.AxisListType.X, ALU.add),
                 [tl], [tl])
            P.op("vector", lambda e, l_=l_: e.reciprocal(l_[:, 42:43], l_[:, 41:42]), [tl], [tl])
            P.ts(gall[:, c, :], l_[:, 24:32], l_[:, 42:43], None, ALU.mult, None, [tl], [t_gall])

    run_pipeline(chunk, NCH, 2, DEP)
    if moe:
        P.dma("sync", gates_dst, gall[:, :, :], "b_g", [t_gall], [])


D_IN_PROJ = 4624
NOMOE = False
NFILL = 0
MOE_STEPS = 3
PAIRS = [[0, 1], [2, 3], [4, 5], [6, 7]]
WNAMES = [("norm_mix_g", [2, 1024]), ("w_in", [2, 1024, D_IN_PROJ]), ("conv_w", [2, 4, 1536]), ("conv_b", [2, 1536]),
          ("dt_bias", [2, 16]), ("a_log", [2, 16]), ("d_skip", [2, 16]), ("ssd_norm_g", [2, 1024]),
          ("sgu_norm_g", [2, 1024]), ("sgu_norm_b", [2, 1024]), ("wsT", [2, 128, 8, 128]), ("bspT", [2, 128, 8]),
          ("sgu_out_g", [2, 1024]), ("w_out", [2, 2048, 1024]), ("norm_ffn_g", [2, 1024]),
          ("ffn_w_gate", [1, 1024, D_FF]), ("ffn_w_up", [1, 1024, D_FF]), ("ffn_w_down", [1, D_FF, 1024]),
          ("wr_l", [128, 8, 8]), ("moe_w_gate", [1, 8, 1024, D_FF]), ("moe_w_up", [1, 8, 1024, D_FF]),
          ("moe_w_down", [1, 8, D_FF, 1024]), ("final_norm_g", [1024])]


def build_program(T_tok=4096, TH=2048, layers=(0, 1), n_exp=N_EXPERTS, d_ff=D_FF, stop_after=None, dbg=False):
    nc = bass.Bass("TRN2", target_bir_lowering=False)
    I = {}
    I["x"] = nc.dram_tensor("x", [T_tok, 1024], F32, kind="ExternalInput").ap()
    for n, shp in WNAMES:
        shp = list(shp)
        if n in ("ffn_w_gate", "ffn_w_up"):
            shp[2] = d_ff
        if n == "ffn_w_down":
            shp[1] = d_ff
        if n in ("moe_w_gate", "moe_w_up"):
            shp[3] = d_ff
        if n == "moe_w_down":
            shp[2] = d_ff
        I[n] = nc.dram_tensor(n, shp, F32, kind="ExternalInput").ap()
    I["flag"] = nc.dram_tensor("flag", [128, 1], F32, kind="ExternalInput").ap()
    I["cf"] = nc.dram_tensor("cf", [128, 384], F32, kind="ExternalInput").ap()
    I["cb"] = nc.dram_tensor("cb", [128, 1792], BF16, kind="ExternalInput").ap()
    out = nc.dram_tensor("out", [T_tok, 1024], F32, kind="ExternalOutput").ap()
    hA = nc.dram_tensor("hA", [T_tok, 1024], F32).ap()
    hB = nc.dram_tensor("hB", [T_tok, 1024], F32).ap()
    hnTa = nc.dram_tensor("hnTa", [8, 128, T_tok], BF16).ap()
    hnTb = nc.dram_tensor("hnTb", [8, 128, T_tok], BF16).ap()
    yssd = nc.dram_tensor("yssd", [T_tok, 1024], BF16).ap()
    gates = nc.dram_tensor("gates", [128, T_tok // 128, 8], F32).ap()
    cc_in = [nc.dram_tensor("cc_in%d" % L, [131, 1536], F32).ap() for L in range(2)]
    cc_out = [nc.dram_tensor("cc_out%d" % L, [2 * 131, 1536], F32).ap() for L in range(2)]
    with contextlib.ExitStack() as gs:
        P = Prog(nc, gs)
        C = Ctx(nc, gs, P)
        C.psum = gs.enter_context(nc.psum_tensor("psum", [128, 8, 512], F32))
        C.t_ps = [T("ps%d" % b) for b in range(8)]
        K = load_consts(C, I["cf"], I["cb"])
        C.t_const = K.t

        def phase(fn):
            with contextlib.ExitStack() as ps:
                C.stack = ps
                C.t_ps = [T("ps%d" % b) for b in range(8)]
                fn()
                P.emit()

        def done(tag, src):
            return stop_after == tag

        stopped = False
        for L in layers:
            if stopped:
                break
            src = I["x"] if L == layers[0] else hB
            a_args = (I["w_in"][L], I["norm_mix_g"][L], I["conv_w"][L], I["conv_b"][L], I["dt_bias"][L],
                      I["a_log"][L], I["d_skip"][L], I["ssd_norm_g"][L])
            phase(lambda: mixer_A(C, K, L, T_tok, src, *a_args, True, None, cc_in[L], I["flag"], None, None))
            P.op("gpsimd", lambda e, L=L: e.collective_compute("AllGather", ALU.bypass, replica_groups=PAIRS,
                                                              ins=[cc_in[L]], outs=[cc_out[L]]),
                 [], [], dma_key="cc", inc=1)
            P.emit()
            phase(lambda: mixer_A(C, K, L, T_tok, src, *a_args, False, cc_out[L], None, I["flag"], hnTa, yssd))
            moe = (L % 2 == 1) and not NOMOE
            dstB = hA
            phase(lambda: mixer_B(C, K, L, T_tok, src, dstB, hnTa, yssd, I["w_in"][L], I["sgu_norm_g"][L],
                                  I["sgu_norm_b"][L], I["wsT"][L], I["bspT"][L], I["sgu_out_g"][L], I["w_out"][L],
                                  I["norm_ffn_g"][L], hnTb, I["wr_l"] if moe else None,
                                  gates if moe else None))
            if stop_after == "mix%d" % L:
                fin_dst, stopped = hA, True
                break
            last = (L == layers[-1]) and (L == 1)
            if not moe:
                wl = [(I["ffn_w_gate"][0], I["ffn_w_up"][0], I["ffn_w_down"][0], None)]
                gsrc = None
            else:
                wl = [(I["moe_w_gate"][0, e], I["moe_w_up"][0, e], I["moe_w_down"][0, e], e) for e in range(n_exp)]
                gsrc = gates
            dstF = out if last else hB
            phase(lambda: ffn_phase(C, "F%d_" % L, T_tok, TH, hA, hnTb, gsrc, wl, dstF,
                                    fin_g=I["final_norm_g"] if last else None))
            if stop_after == "ffn%d" % L and not last:
                fin_dst, stopped = hB, True
                break
        if stopped:
            def cp():
                tmp = C.sb("dbgcp", [128, T_tok // 128, 1024], F32)
                tt_ = T("dbg")
                P.dma("sync", tmp[:, :, :], fin_dst.rearrange("(j p) d -> p j d", p=128), "dbg", [], [tt_])
                P.dma("sync", out.rearrange("(j p) d -> p j d", p=128), tmp[:, :, :], "dbg", [tt_], [])
            phase(cp)
    return nc


_NC_CACHE = {}


def make_in_maps(inputs, n_cores=8, T_tok=4096):
    cf, cb = host_consts()
    x = np.asarray(inputs["x"], dtype=np.float32)
    B, S, D = x.shape
    halves = S // T_tok
    shared = {}
    for n, _ in WNAMES:
        if n == "wsT":
            shared[n] = np.ascontiguousarray(np.asarray(inputs["w_spatial"], np.float32).transpose(0, 3, 1, 2))
        elif n == "wr_l":
            shared[n] = np.ascontiguousarray(
                np.asarray(inputs["moe_w_router"], np.float32)[0].reshape(8, 128, 8).transpose(1, 0, 2))
        elif n == "bspT":
            shared[n] = np.ascontiguousarray(np.asarray(inputs["b_spatial"], np.float32).transpose(0, 2, 1))
        else:
            shared[n] = np.ascontiguousarray(np.asarray(inputs[n], np.float32))
    shared["cf"] = cf
    shared["cb"] = cb
    maps = []
    for c in range(n_cores):
        b, hf = c // halves, c % halves
        m = dict(shared)
        m["x"] = np.ascontiguousarray(x[b, hf * T_tok:(hf + 1) * T_tok])
        m["flag"] = np.full((128, 1), float(hf), np.float32)
        maps.append(m)
    return maps


def kernel(**inputs):
    if "full" not in _NC_CACHE:
        _NC_CACHE["full"] = build_program()
    nc = _NC_CACHE["full"]
    maps = make_in_maps(inputs)
    res = run_bass_kernel_spmd(nc, maps, core_ids=list(range(8)))
    x = inputs["x"]
    B, S, D = x.shape
    outp = np.empty((B, S, D), np.float32)
    for c in range(8):
        b, hf = c // 2, c % 2
        outp[b, hf * 4096:(hf + 1) * 4096] = res.results[c]["out"]
    return outp
```

```python
import contextlib
import numpy as np
import concourse.bass as bass
import concourse.mybir as mybir
from concourse.bass_utils import run_bass_kernel_spmd

F32 = mybir.dt.float32
BF16 = mybir.dt.bfloat16
AF = mybir.ActivationFunctionType
ALU = mybir.AluOpType

D_MODEL = 1024
D_FF = 3584
N_EXPERTS = 8
ENGS = ("tensor", "vector", "scalar", "gpsimd", "sync")


class T:
    __slots__ = ("name", "last_w", "readers")

    def __init__(self, name):
        self.name = name
        self.last_w = None
        self.readers = {}


class Op:
    __slots__ = ("eng", "fn", "deps", "flag", "val", "sem", "is_dma", "key", "inc", "blk")


class Prog:
    def __init__(self, nc, gstack, same_eng_sync=True):
        self.nc = nc
        self.gstack = gstack
        self.base_waited = {}
        self.ops = {e: [] for e in ENGS}
        self.same_eng_sync = same_eng_sync
        self.dma_cnt = {}
        self.nops = 0
        self.blk = 0
        self.fill = None
        self.last_dma = {}

    def op(self, eng, fn, reads=(), writes=(), dma_key=None, inc=16):
        o = Op()
        o.inc = inc
        o.blk = self.blk
        o.eng = eng
        o.fn = fn
        o.flag = False
        o.val = 0
        o.sem = None
        o.is_dma = dma_key is not None
        o.key = dma_key
        deps = {}
        for t in reads:
            if t.last_w is not None:
                deps[id(t.last_w)] = t.last_w
        for t in writes:
            if t.last_w is not None:
                deps[id(t.last_w)] = t.last_w
            for r in t.readers.values():
                deps[id(r)] = r
        if dma_key is not None:
            prev = self.last_dma.get(dma_key)
            if prev is not None:
                deps[id(prev)] = prev
            self.last_dma[dma_key] = o
        dl = []
        for d in deps.values():
            if d is o or d.blk != self.blk:
                continue
            if (not d.is_dma) and d.eng == eng and not o.is_dma:
                if eng == "tensor" or not self.same_eng_sync:
                    continue
            dl.append(d)
        o.deps = dl
        if eng == "tensor" and self.fill is not None and dl and not getattr(self, "_in_fill", False):
            self._in_fill = True
            n, f_out, f_l, f_r = self.fill
            for _ in range(n):
                self.op("tensor", lambda e: e.matmul(f_out, f_l, f_r, start=True, stop=True), [], [])
            self._in_fill = False
        rk = (eng, dma_key)
        for t in reads:
            t.readers[rk] = o
        for t in writes:
            t.last_w = o
            t.readers = {}
        self.ops[eng].append(o)
        self.nops += 1
        return o

    def mm(self, out, lhsT, rhs, start, stop, reads, writes):
        return self.op("tensor", lambda e: e.matmul(out, lhsT, rhs, start=start, stop=stop), reads, writes)

    def tr(self, out, in_, ident, reads, writes):
        return self.op("tensor", lambda e: e.transpose(out, in_, ident), reads, writes)

    def act(self, out, in_, func, reads, writes, bias=None, scale=None, accum_out=None, eng="scalar"):
        kw = {}
        if bias is not None:
            kw["bias"] = bias
        if scale is not None:
            kw["scale"] = scale
        if accum_out is not None:
            kw["accum_out"] = accum_out
        return self.op(eng, lambda e: e.activation(out, in_, func, **kw), reads, writes)

    def tt(self, out, in0, in1, op, reads, writes, eng="vector"):
        return self.op(eng, lambda e: e.tensor_tensor(out, in0, in1, op), reads, writes)

    def ts(self, out, in0, s1, s2, op0, op1, reads, writes, eng="vector", accum_out=None):
        if op1 is None:
            return self.op(eng, lambda e: e.tensor_scalar(out, in0, s1, None, op0), reads, writes)
        if accum_out is not None:
            return self.op(eng, lambda e: e.tensor_scalar(out, in0, s1, s2, op0, op1, accum_out=accum_out), reads, writes)
        return self.op(eng, lambda e: e.tensor_scalar(out, in0, s1, s2, op0, op1), reads, writes)

    def stt(self, out, in0, scalar, in1, op0, op1, reads, writes):
        return self.op("vector", lambda e: e.scalar_tensor_tensor(out, in0, scalar, in1, op0, op1), reads, writes)

    def copy(self, out, in_, reads, writes, eng="vector"):
        if eng == "scalar":
            return self.act(out, in_, AF.Copy, reads, writes)
        return self.op(eng, lambda e: e.tensor_copy(out, in_), reads, writes)

    def dma(self, eng, out, in_, key, reads, writes, **kw):
        return self.op(eng, lambda e: e.dma_start(out=out, in_=in_, **kw), reads, writes, dma_key=key)

    def emit(self, stack=None):
        nc = self.nc
        gs = self.gstack
        if not hasattr(self, "sems"):
            self.sems = {e: gs.enter_context(nc.semaphore("s_" + e)) for e in ENGS}
            self.keysem = {}
            self.eng_cnt = {e: 0 for e in ENGS}
        sems, keysem = self.sems, self.keysem
        for e in ENGS:
            for o in self.ops[e]:
                for d in o.deps:
                    d.flag = True
        for e in ENGS:
            c = self.eng_cnt[e]
            for o in self.ops[e]:
                if o.is_dma:
                    if o.key not in keysem:
                        keysem[o.key] = gs.enter_context(nc.semaphore("d_" + o.key))
                        self.dma_cnt[o.key] = 0
                    self.dma_cnt[o.key] += o.inc
                    o.val = self.dma_cnt[o.key]
                    o.sem = keysem[o.key]
                elif o.flag:
                    c += 1
                    o.val = c
                    o.sem = sems[e]
            self.eng_cnt[e] = c
        prog = self
        base_waited = dict(self.base_waited)

        def body(ename):
            def _f(eng):
                waited = dict(base_waited)
                for o in prog.ops[ename]:
                    need = {}
                    for d in o.deps:
                        k = id(d.sem)
                        if k not in need or need[k][1] < d.val:
                            need[k] = (d.sem, d.val)
                    for k, (s, v) in need.items():
                        if waited.get(k, 0) < v:
                            eng.wait_ge(s, v)
                            waited[k] = v
                    inst = o.fn(eng)
                    if o.is_dma:
                        if o.inc == 16:
                            inst.then_inc(o.sem, 16)
                        else:
                            inst.then_inc(o.sem)
                    elif o.flag:
                        inst.then_inc(o.sem, 1)
                if ename == "sync":
                    for k, s in keysem.items():
                        eng.wait_ge(s, prog.dma_cnt[k])
            return _f

        with nc.Block() as block:
            block.tensor(body("tensor"))
            block.vector(body("vector"))
            block.scalar(body("scalar"))
            block.gpsimd(body("gpsimd"))
            block.sync(body("sync"))
        for e in ENGS:
            self.base_waited[id(sems[e])] = self.eng_cnt[e]
        for k, sm in keysem.items():
            self.base_waited[id(sm)] = self.dma_cnt[k]
        self.ops = {e: [] for e in ENGS}
        self.blk += 1


class Ctx:
    def __init__(self, nc, stack, prog):
        self.nc = nc
        self.stack = stack
        self.P = prog
        self._n = 0

    def sb(self, name, shape, dtype):
        return self.stack.enter_context(self.nc.sbuf_tensor(name, list(shape), dtype))

    def ps(self, name, shape, dtype):
        return self.stack.enter_context(self.nc.psum_tensor(name, list(shape), dtype))

    def dram(self, name, shape, dtype, kind="Internal"):
        return self.nc.dram_tensor(name, list(shape), dtype, kind=kind).ap()


def ffn_phase(C, tag, T_tok, TH, h_src, hnT_src, gates_src, wlist, h_dst, fin_g=None,
              h_src_tiles=None, hnT_tiles=None, gates_tiles=None, h_dst_tiles=None, FC=512, emit_every=2):
    P = C.P
    NJ = TH // 128
    NTT = TH // 512
    NFS = FC // 128
    acc = C.sb(tag + "acc", [128, NJ, 1024], F32)
    hnT = C.sb(tag + "hnT", [128, 8, TH], BF16)
    wg = [C.sb(tag + "wg%d" % s, [128, 8, FC], BF16) for s in range(2)]
    wu = [C.sb(tag + "wu%d" % s, [128, 8, FC], BF16) for s in range(2)]
    wd = [C.sb(tag + "wd%d" % s, [128, NFS, 1024], BF16) for s in range(2)]
    actb = [C.sb(tag + "act%d" % s, [128, NFS, 512], BF16) for s in range(2)]
    sgb = [C.sb(tag + "sg%d" % s, [128, 512], F32) for s in range(2)]
    gts = C.sb(tag + "gts", [128, NJ, 8], F32) if gates_src is not None else None
    psum = C.psum
    t_acc = [T("acc%d" % j) for j in range(NJ)]
    t_hnT = [T("hnT%d" % j) for j in range(NTT)]
    t_wg = [T("wg%d" % s) for s in range(2)]
    t_wu = [T("wu%d" % s) for s in range(2)]
    t_wd = [T("wd%d" % s) for s in range(2)]
    t_act = [T("act%d" % s) for s in range(2)]
    t_sg = [T("sg%d" % s) for s in range(2)]
    t_gts = T("gts")
    t_ps = C.t_ps
    if fin_g is not None:
        fgrow = C.sb(tag + "fgrow", [128, 1024], F32)
        t_fg = T("fgrow")
        P.dma("sync", fgrow[:, :], fin_g.partition_broadcast(128), "ffn_fg", [], [t_fg])
        fscr = C.sb(tag + "fscr", [128, 1024], BF16)
        fss = C.sb(tag + "fss", [128, 2], F32)
        fout = [C.sb(tag + "fout%d" % s, [128, 1024], F32) for s in range(2)]
        t_fscr, t_fss = T("fscr"), T("fss")
        t_fout = [T("fout%d" % s) for s in range(2)]

    nhalf = T_tok // TH
    wci = 0
    for half in range(nhalf):
        t0 = half * TH
        for j in range(NJ):
            rd = [h_src_tiles[(t0 // 128) + j]] if h_src_tiles else []
            P.dma("sync", acc[:, j, :], h_src[t0 + j * 128: t0 + (j + 1) * 128, :], "ffn_acc%d" % j,
                  rd, [t_acc[j]])
        for tt in range(NTT):
            rd = [hnT_tiles[(t0 // 128) + tt * 4 + q] for q in range(4)] if hnT_tiles else []
            P.dma("sync", hnT[:, :, tt * 512:(tt + 1) * 512],
                  hnT_src[:, :, t0 + tt * 512: t0 + (tt + 1) * 512].rearrange("k p t -> p k t"),
                  "ffn_hnT%d" % tt, rd, [t_hnT[tt]])
        if gts is not None:
            rd = [gates_tiles[(t0 // 128) + j] for j in range(NJ)] if gates_tiles else []
            P.dma("sync", gts[:, :, :], gates_src[:, t0 // 128:(t0 + TH) // 128, :],
                  "ffn_gts", rd, [t_gts])

        pend = None

        def down(slot, tt, gi, aslot):
            for st in range(4):
                j = tt * 4 + st
                pb = 4 + 2 * (j % 2)
                for hd in range(2):
                    for fs in range(NFS):
                        P.mm(psum[:, pb + hd, :], actb[aslot][:, fs, st * 128:(st + 1) * 128],
                             wd[slot][:, fs, hd * 512:(hd + 1) * 512], fs == 0, fs == NFS - 1,
                             [t_act[aslot], t_wd[slot]], [t_ps[pb + hd]])
                po = psum[:, pb:pb + 2, :]
                av = acc[:, j, :].rearrange("p (a b) -> p a b", a=2)
                if gi is None:
                    P.tt(av, po, av, ALU.add, [t_ps[pb], t_ps[pb + 1], t_acc[j]], [t_acc[j]])
                else:
                    P.stt(av, po, gts[:, j, gi:gi + 1], av, ALU.mult, ALU.add,
                          [t_ps[pb], t_ps[pb + 1], t_acc[j], t_gts], [t_acc[j]])

        tti = 0
        for wi, (Wg, Wu, Wd, gi) in enumerate(wlist):
            if wi > 0 and emit_every and wi % emit_every == 0:
                down(*pend)
                pend = None
                P.emit()
            Fdim = Wg.shape[1]
            for fc in range(Fdim // FC):
                slot = wci % 2
                wci += 1
                P.dma("gpsimd", wg[slot][:, :, :],
                      Wg[:, fc * FC:(fc + 1) * FC].rearrange("(k p) f -> p k f", p=128),
                      "w_g%d" % slot, [], [t_wg[slot]])
                P.dma("gpsimd", wu[slot][:, :, :],
                      Wu[:, fc * FC:(fc + 1) * FC].rearrange("(k p) f -> p k f", p=128),
                      "w_u%d" % slot, [], [t_wu[slot]])
                P.dma("gpsimd", wd[slot][:, :, :],
                      Wd[fc * FC:(fc + 1) * FC, :].rearrange("(s p) d -> p s d", p=128),
                      "w_d%d" % slot, [], [t_wd[slot]])
                for tt in range(NTT):
                    aslot = tti % 2
                    tti += 1
                    for fs in range(NFS):
                        b = fs % 2
                        pg, pu = psum[:, b, :], psum[:, 2 + b, :]
                        for kc in range(8):
                            P.mm(pg, wg[slot][:, kc, fs * 128:(fs + 1) * 128], hnT[:, kc, tt * 512:(tt + 1) * 512],
                                 kc == 0, kc == 7, [t_wg[slot], t_hnT[tt]], [t_ps[b]])
                        for kc in range(8):
                            P.mm(pu, wu[slot][:, kc, fs * 128:(fs + 1) * 128], hnT[:, kc, tt * 512:(tt + 1) * 512],
                                 kc == 0, kc == 7, [t_wu[slot], t_hnT[tt]], [t_ps[2 + b]])
                        P.act(sgb[b][:, :], pg, AF.Silu, [t_ps[b]], [t_sg[b]])
                        P.tt(actb[aslot][:, fs, :], pu, sgb[b][:, :], ALU.mult,
                             [t_ps[2 + b], t_sg[b]], [t_act[aslot]])
                    if pend is not None:
                        down(*pend)
                    pend = (slot, tt, gi, aslot)
        down(*pend)
        pend = None
        for j in range(NJ):
            jj = (t0 // 128) + j
            if fin_g is None:
                wr = [h_dst_tiles[jj]] if h_dst_tiles else []
                P.dma("sync", h_dst[t0 + j * 128: t0 + (j + 1) * 128, :], acc[:, j, :], "ffn_st%d" % (j % 2),
                      [t_acc[j]], wr)
            else:
                s = j % 2
                P.act(fscr[:, :], acc[:, j, :], AF.Square, [t_acc[j]], [t_fscr, t_fss], accum_out=fss[:, 0:1])
                rstd_ops(P, fss[:, 1:2], fss[:, 0:1], 1.0 / 1024, [t_fss], [t_fss])
                P.stt(fout[s][:, :], acc[:, j, :], fss[:, 1:2], fgrow[:, :], ALU.mult, ALU.mult,
                      [t_acc[j], t_fss, t_fg], [t_fout[s]])
                P.dma("sync", h_dst[t0 + j * 128: t0 + (j + 1) * 128, :], fout[s][:, :], "ffn_st%d" % s,
                      [t_fout[s]], [])


def rstd_ops(P, out, ss, inv_n, reads, writes, eps=1e-6):
    P.ts(out, ss, inv_n, eps, ALU.mult, ALU.add, reads, writes)
    P.act(out, out, AF.Sqrt, reads, writes)
    P.op("vector", lambda e: e.reciprocal(out, out), reads, writes)


def host_consts():
    import ml_dtypes
    i = np.arange(128)
    ident = np.eye(128, dtype=np.float32)
    triU = (i[:, None] <= i[None, :]).astype(np.float32)
    ones = np.ones((128, 128), np.float32)
    cf = np.concatenate([ident, triU, ones], axis=1)
    sc = [(i[:, None] == (i[None, :] - (3 - k))).astype(np.float32) for k in range(3)]
    sp = [(i[:, None] == (128 + i[None, :] - (3 - k))).astype(np.float32) for k in range(3)]
    mneg = np.where(i[None, :] < i[:, None], -30000.0, 0.0).astype(np.float32)
    mneg4 = np.tile(mneg, (1, 4))
    onesrow = np.zeros((128, 128), np.float32)
    onesrow[0, :] = 1.0
    cb = np.concatenate([ident] + sc + sp + [mneg4, onesrow, ones, triU], axis=1).astype(ml_dtypes.bfloat16)
    return cf, cb


class Consts:
    pass


def load_consts(C, cf_ap, cb_ap):
    P = C.P
    K = Consts()
    gs = C.P.gstack
    cf = gs.enter_context(C.nc.sbuf_tensor("cst_f", [128, 384], F32))
    cb = gs.enter_context(C.nc.sbuf_tensor("cst_b", [128, 1792], BF16))
    K.t = T("consts")
    P.dma("sync", cf[:, :], cf_ap, "const", [], [K.t])
    P.dma("sync", cb[:, :], cb_ap, "const", [], [K.t])
    K.identF = cf[:, 0:128]
    K.triU = cf[:, 128:256]
    K.onesF = cf[:, 256:384]
    K.identB = cb[:, 0:128]
    K.Sc = [cb[:, 128 * (1 + k):128 * (2 + k)] for k in range(3)]
    K.Sp = [cb[:, 128 * (4 + k):128 * (5 + k)] for k in range(3)]
    K.mneg = cb[:, 896:1408]
    K.onesrow = cb[0:1, 1408:1536]
    K.onesB = cb[:, 1536:1664]
    K.triUB = cb[:, 1664:1792]
    return K


def rstd_from_ss(P, st, i_ss, i_out, inv_n, tl, eps=1e-6):
    rstd_ops(P, st[:, i_out:i_out + 1], st[:, i_ss:i_ss + 1], inv_n, [tl], [tl], eps)


def mixer_A(C, K, L, T_tok, h_src, w_in, norm_g, conv_w, conv_b, dt_bias, a_log, d_skip, ssd_g,
            state_only, st_in, st_out, flag_ap, hnT_dst, yssd_dst):
    P = C.P
    NCH = T_tok // 128
    tg = "A%d%s_" % (L, "p" if state_only else "m")
    sb = C.sb
    NW = 2576
    win = sb(tg + "win", [128, 8, NW], BF16)
    gmix = sb(tg + "gmix", [128, 1024], F32)
    gssd = sb(tg + "gssd", [128, 1024], F32)
    cw = sb(tg + "cw", [128, 4, 1536], BF16)
    cbias = sb(tg + "cbias", [1, 1536], BF16)
    sm = sb(tg + "sm", [128, 64], F32)
    t_win, t_row, t_sm = T("win"), T("rows"), T("sm")
    blocks = [(0, 512), (512, 512), (1024, 512), (1536, 512), (2048, 512), (2560, 16)]
    for (c0, cn) in blocks:
        P.dma("gpsimd", win[:, :, c0:c0 + cn], w_in[:, c0:c0 + cn].rearrange("(k p) f -> p k f", p=128),
              "cw_a_w", [], [t_win])
    P.dma("sync", gmix[:, :], norm_g.partition_broadcast(128), "cr_a", [], [t_row])
    P.dma("sync", gssd[:, :], ssd_g.partition_broadcast(128), "cr_a", [], [t_row])
    for k in range(4):
        P.dma("gpsimd", cw[:, k, :], conv_w[k, :].partition_broadcast(128), "cw_a", [], [t_row])
    P.dma("gpsimd", cbias[:, :], conv_b.unsqueeze(0), "cw_a", [], [t_row])
    P.dma("sync", sm[:, 0:16], dt_bias.partition_broadcast(128), "cr_a", [], [t_sm])
    P.dma("sync", sm[:, 16:32], a_log.partition_broadcast(128), "cr_a", [], [t_sm])
    P.dma("sync", sm[:, 32:48], d_skip.partition_broadcast(128), "cr_a", [], [t_sm])
    P.dma("sync", sm[:, 48:49], flag_ap, "cr_a", [], [t_sm])
    P.act(sm[:, 16:32], sm[:, 16:32], AF.Exp, [t_sm], [t_sm])
    P.ts(sm[:, 16:32], sm[:, 16:32], -1.0, None, ALU.mult, None, [t_sm], [t_sm])
    dtb, arow, dsk, flg = sm[:, 0:16], sm[:, 16:32], sm[:, 32:48], sm[:, 48:49]

    DEP = 3 if state_only else 2

    def two(name, shape, dt):
        return [sb(tg + name + "%d" % s, shape, dt) for s in range(DEP)]

    def twoT(name):
        return [T(name + "%d" % s) for s in range(DEP)]

    hin = two("hin", [128, 1024], F32)
    scr_a = sb(tg + "scra", [128, 1024], BF16)
    st_a = sb(tg + "sta", [128, 4], F32)
    hn = sb(tg + "hn", [128, 1024], F32)
    hnT = two("hnT", [128, 8, 128], BF16)
    raw = sb(tg + "raw", [128, 1536], BF16)
    rw = two("rw", [128, 4, 1536], BF16)
    xa = two("xa", [128, 1536], BF16)
    dts = two("dts", [128, 128], F32)
    xdtd = two("xdtd", [128, 1024], BF16)
    S = sb(tg + "S", [128, 1024], F32)
    t_hin, t_hnT, t_rw, t_xa, t_dts, t_xdtd = [twoT(n) for n in ("hin", "hnT", "rw", "xa", "dts", "xdtd")]
    t_scra, t_sta, t_hn, t_raw, t_S = [T(n) for n in ("scra", "sta", "hn", "raw", "S")]
    t_ps = C.t_ps
    psum = C.psum
    if not state_only:
        scr_b = sb(tg + "scrb", [128, 1024], BF16)
        st_b = sb(tg + "stb", [128, 4], F32)
        sz = two("sz", [128, 1024], BF16)
        ncs = two("ncs", [128, 32], F32)
        rhs1 = sb(tg + "rhs1", [128, 2, 16, 128], BF16)
        dab = sb(tg + "dab", [128, 32], BF16)
        bct = two("bct", [128, 4, 128], BF16)
        cbT = two("cbT", [128, 2, 128], BF16)
        decT = two("decT", [128, 16, 128], BF16)
        scT = two("scT", [128, 16, 128], BF16)
        xdt = two("xdt", [128, 1024], BF16)
        xds = two("xds", [128, 1024], BF16)
        Sb = two("Sb", [128, 1024], BF16)
        y1 = sb(tg + "y1", [128, 1024], F32)
        y2 = sb(tg + "y2", [128, 1024], F32)
        yn = two("yn", [128, 1024], BF16)
        t_sz, t_ncs, t_bct, t_cbT, t_decT, t_scT, t_xdt, t_xds, t_Sb, t_yn = [twoT(n) for n in (
            "sz", "ncs", "bct", "cbT", "decT", "scT", "xdt", "xds", "Sb", "yn")]
        t_scrb, t_stb, t_rhs1, t_y1, t_y2 = [T(n) for n in ("scrb", "stb", "rhs1", "y1", "y2")]

    if state_only:
        P.op("gpsimd", lambda e: e.memset(S[:, :], 0.0), [], [t_S])
        P.op("gpsimd", lambda e: e.memset(rw[DEP - 1][:, :, :], 0.0), [], [t_rw[DEP - 1]])
    else:
        P.dma("sync", S[:, :], st_in[0:128, 0:1024], "st_ld", [], [t_S])
        P.ts(S[:, :], S[:, :], flg, None, ALU.mult, None, [t_S, t_sm], [t_S])
        P.copy(Sb[0][:, :], S[:, :], [t_S], [t_Sb[0]], eng="scalar")
        rawh = y1
        P.op("gpsimd", lambda e: e.memset(rawh[:, :], 0.0), [], [t_y1])
        P.dma("sync", rawh[125:128, 0:768], st_in[128:131, 0:768], "st_ld2", [t_y1], [t_y1])
        P.ts(rawh[:, 0:768], rawh[:, 0:768], flg, None, ALU.mult, None, [t_y1, t_sm], [t_y1])
        for k in range(3):
            P.tt(rw[DEP - 1][:, k, 0:768], rawh[:, 0:768], cw[:, k, 0:768], ALU.mult, [t_y1, t_row], [t_rw[DEP - 1]],
                 eng="gpsimd")
        P.op("gpsimd", lambda e: e.memset(rawh[:, :], 0.0), [t_y1], [t_y1]) if False else None
        P.dma("sync", rawh[125:128, 0:768], st_in[128:131, 768:1536], "st_ld2", [t_y1, t_rw[DEP - 1]], [t_y1])
        P.ts(rawh[:, 0:768], rawh[:, 0:768], flg, None, ALU.mult, None, [t_y1, t_sm], [t_y1])
        for k in range(3):
            P.tt(rw[DEP - 1][:, k, 768:1536], rawh[:, 0:768], cw[:, k, 768:1536], ALU.mult, [t_y1, t_row],
                 [t_rw[DEP - 1]], eng="gpsimd")

    ps_hn = psum[:, 0:2, :].rearrange("p a (k t) -> p (a k) t", k=4)
    h16 = lambda ap: ap.rearrange("p (h d) -> p h d", h=16)
    pbs = [2, 3]

    def chunk(c):
        s2 = c % DEP
        sp = (c - 1) % DEP
        sn = (c + 1) % DEP
        tok = slice(c * 128, (c + 1) * 128)
        d_ = dts[s2]
        td = t_dts[s2]
        for cc in ([0, 1] if c == 0 else [c + 1]):
            if cc < NCH:
                P.dma("sync", hin[cc % DEP][:, :], h_src[cc * 128:(cc + 1) * 128, :], "a_h%d" % (cc % DEP), [],
                      [t_hin[cc % DEP]])
        P.act(scr_a[:, :], hin[s2][:, :], AF.Square, [t_hin[s2]], [t_scra, t_sta], accum_out=st_a[:, 0:1])
        rstd_from_ss(P, st_a, 0, 1, 1.0 / 1024, t_sta)
        P.stt(hn[:, :], hin[s2][:, :], st_a[:, 1:2], gmix[:, :], ALU.mult, ALU.mult,
              [t_hin[s2], t_sta, t_row], [t_hn])
        yield
        for k in range(8):
            P.tr(ps_hn[:, k, :], hn[:, k * 128:(k + 1) * 128], K.identF, [t_hn, K.t], [t_ps[k // 4]])
        P.copy(hnT[s2][:, :, :], ps_hn, [t_ps[0], t_ps[1]], [t_hnT[s2]], eng="scalar")
        if not state_only:
            P.dma("scalar", hnT_dst[:, :, tok].rearrange("k p t -> p k t"), hnT[s2][:, :, :], "a_hnT%d" % s2,
                  [t_hnT[s2]], [])
        bi = 0
        for (c0, cn) in blocks:
            if c0 >= 1024 or state_only:
                continue
            pb = pbs[bi % 2]
            bi += 1
            for k in range(8):
                P.mm(psum[:, pb, 0:cn], hnT[s2][:, k, :], win[:, k, c0:c0 + cn], k == 0, k == 7,
                     [t_hnT[s2], t_win], [t_ps[pb]])
            P.act(sz[s2][:, c0:c0 + cn], psum[:, pb, 0:cn], AF.Silu, [t_ps[pb]], [t_sz[s2]])
        yield
        lite = state_only and c != NCH - 1
        for (c0, cn) in blocks:
            if c0 < 1024:
                continue
            if lite and c0 == 2048:
                cn = 256
            pb = pbs[bi % 2]
            bi += 1
            for k in range(8):
                P.mm(psum[:, pb, 0:cn], hnT[s2][:, k, :], win[:, k, c0:c0 + cn], k == 0, k == 7,
                     [t_hnT[s2], t_win], [t_ps[pb]])
            if c0 < 2560:
                P.copy(raw[:, c0 - 1024:c0 - 1024 + cn], psum[:, pb, 0:cn], [t_ps[pb]], [t_raw], eng="scalar")
            else:
                P.tt(d_[:, 0:16], psum[:, pb, 0:16], dtb, ALU.add, [t_ps[pb], t_sm], [td])
        P.act(d_[:, 16:32], d_[:, 0:16], AF.Exp, [td], [td])
        P.act(d_[:, 32:48], d_[:, 16:32], AF.Ln, [td], [td], bias=1.0)
        P.tt(d_[:, 48:64], d_[:, 32:48], arow, ALU.mult, [td, t_sm], [td])
        ncv = 1280 if lite else 1536
        for k in range(4):
            P.tt(rw[s2][:, k, 0:ncv], raw[:, 0:ncv], cw[:, k, 0:ncv], ALU.mult, [t_raw, t_row], [t_rw[s2]])
        yield
        for b3 in range(3):
            pb = pbs[b3 % 2]
            wd_ = 256 if (state_only and b3 == 2) else 512
            cs_ = slice(b3 * 512, b3 * 512 + wd_)
            po_ = psum[:, pb, 0:wd_]
            P.mm(po_, K.onesrow, cbias[0:1, cs_], True, False, [K.t, t_row], [t_ps[pb]])
            for k in range(3):
                P.mm(po_, K.Sp[k], rw[sp][:, k, cs_], False, False, [K.t, t_rw[sp]], [t_ps[pb]])
            for k in range(3):
                P.mm(po_, K.Sc[k], rw[s2][:, k, cs_], False, False, [K.t, t_rw[s2]], [t_ps[pb]])
            P.mm(po_, K.identB, rw[s2][:, 3, cs_], False, True, [K.t, t_rw[s2]], [t_ps[pb]])
            P.act(xa[s2][:, cs_], po_, AF.Silu, [t_ps[pb]], [t_xa[s2]])
        P.mm(psum[:, 4, 0:16], K.triU, d_[:, 48:64], True, True, [K.t, td], [t_ps[4]])
        P.mm(psum[:, 4, 16:32], K.onesF, d_[:, 48:64], True, True, [K.t, td], [t_ps[4]])
        P.copy(d_[:, 64:96], psum[:, 4, 0:32], [t_ps[4]], [td])
        P.tt(d_[:, 96:112], d_[:, 80:96], d_[:, 64:80], ALU.subtract, [td], [td])
        P.act(d_[:, 96:112], d_[:, 96:112], AF.Exp, [td], [td])
        P.act(d_[:, 112:128], d_[:, 80:96], AF.Exp, [td], [td])
        yield
        x3 = h16(xa[s2][:, 0:1024])
        dt_b = d_[:, 32:48].unsqueeze(2).to_broadcast([128, 16, 64])
        dte_b = d_[:, 96:112].unsqueeze(2).to_broadcast([128, 16, 64])
        ebl_b = d_[:, 112:128].unsqueeze(2).to_broadcast([128, 16, 64])
        if state_only:
            P.tt(h16(xdtd[s2][:, :]), x3, dt_b, ALU.mult, [t_xa[s2], td], [t_xdtd[s2]], eng="gpsimd")
            P.tt(h16(xdtd[s2][:, :]), h16(xdtd[s2][:, :]), dte_b, ALU.mult, [t_xdtd[s2], td], [t_xdtd[s2]], eng="gpsimd")
        else:
            P.tt(h16(xdt[s2][:, :]), x3, dt_b, ALU.mult, [t_xa[s2], td], [t_xdt[s2]], eng="gpsimd")
            P.tt(h16(xdtd[s2][:, :]), h16(xdt[s2][:, :]), dte_b, ALU.mult, [t_xdt[s2], td], [t_xdtd[s2]], eng="gpsimd")
            P.tt(h16(xds[s2][:, :]), x3, dsk.unsqueeze(2).to_broadcast([128, 16, 64]), ALU.mult,
                 [t_xa[s2], t_sm], [t_xds[s2]], eng="gpsimd")
            n_ = ncs[s2]
            P.ts(n_[:, 0:16], d_[:, 64:80], -1.0, None, ALU.mult, None, [td], [t_ncs[s2]])
            P.act(n_[:, 16:32], d_[:, 64:80], AF.Exp, [td], [t_ncs[s2]])
            ps_t = psum[:, 7, 256:512].bitcast(BF16).rearrange("p (a t) -> p a t", a=4)
            for q in range(4):
                P.tr(ps_t[:, q, :], xa[s2][:, 1024 + q * 128:1024 + (q + 1) * 128], K.identB, [t_xa[s2], K.t], [t_ps[7]])
            P.copy(bct[s2][:, :, :], ps_t, [t_ps[7]], [t_bct[s2]])
            for g in range(2):
                P.mm(psum[:, 7, g * 128:(g + 1) * 128], bct[s2][:, g, :], bct[s2][:, 2 + g, :], True, True,
                     [t_bct[s2]], [t_ps[7]])
            P.copy(cbT[s2][:, :, :], psum[:, 7, 0:256].rearrange("p (g l) -> p g l", g=2), [t_ps[7]], [t_cbT[s2]],
                   eng="scalar")
            P.copy(dab[:, 0:16], d_[:, 48:64], [td], [t_rhs1])
            P.tt(dab[:, 16:32], d_[:, 48:64], dab[:, 0:16], ALU.subtract, [td, t_rhs1], [t_rhs1])
            for hl in range(2):
                P.tt(rhs1[:, hl, :, :], K.triUB.unsqueeze(1).to_broadcast([128, 16, 128]),
                     dab[:, hl * 16:(hl + 1) * 16].unsqueeze(2).to_broadcast([128, 16, 128]), ALU.mult,
                     [K.t, t_rhs1], [t_rhs1])
            yield
            for hb in range(2):
                for q in range(2):
                    h0 = hb * 8 + q * 4
                    P.mm(psum[:, 5 + q, :], K.onesB, rhs1[:, 0, h0:h0 + 4, :].rearrange("p h l -> p (h l)"), True, False,
                         [K.t, t_rhs1], [t_ps[5 + q]])
                    P.mm(psum[:, 5 + q, :], K.onesB, rhs1[:, 1, h0:h0 + 4, :].rearrange("p h l -> p (h l)"), False, False,
                         [K.t, t_rhs1], [t_ps[5 + q]])
                    P.mm(psum[:, 5 + q, :], K.identB, K.mneg, False, True, [K.t], [t_ps[5 + q]])
                    for hh in range(4):
                        h = h0 + hh
                        P.act(decT[s2][:, h, :], psum[:, 5 + q, hh * 128:(hh + 1) * 128], AF.Exp,
                              [t_ps[5 + q], t_ncs[s2]], [t_decT[s2]], bias=n_[:, h:h + 1])
                P.tt(scT[s2][:, hb * 8:(hb + 1) * 8, :], decT[s2][:, hb * 8:(hb + 1) * 8, :],
                     cbT[s2][:, hb, :].unsqueeze(1).to_broadcast([128, 8, 128]), ALU.mult,
                     [t_decT[s2], t_cbT[s2]], [t_scT[s2]])
                yield
        S3 = h16(S[:, :])
        if state_only:
            for g in range(2):
                P.mm(psum[:, 5 + g, :], xa[s2][:, 1024 + g * 128:1024 + (g + 1) * 128], xdtd[s2][:, g * 512:(g + 1) * 512],
                     True, True, [t_xa[s2], t_xdtd[s2]], [t_ps[5 + g]])
            P.tt(S3, S3, ebl_b, ALU.mult, [t_S, td], [t_S])
            P.tt(S[:, :].rearrange("p (a b) -> p a b", a=2), S[:, :].rearrange("p (a b) -> p a b", a=2), psum[:, 5:7, :],
                 ALU.add, [t_S, t_ps[5], t_ps[6]], [t_S])
            return
        e_b = ncs[s2][:, 16:32].unsqueeze(2).to_broadcast([128, 16, 64])
        for g in range(2):
            gs_ = slice(g * 512, (g + 1) * 512)
            P.mm(psum[:, 5, :], K.identB, xds[s2][:, gs_], True, False, [K.t, t_xds[s2]], [t_ps[5]])
            for hh in range(8):
                h = g * 8 + hh
                P.mm(psum[:, 5, hh * 64:(hh + 1) * 64], scT[s2][:, h, :], xdt[s2][:, h * 64:(h + 1) * 64], False, hh == 7,
                     [t_scT[s2], t_xdt[s2]], [t_ps[5]])
            P.mm(psum[:, 6, :], bct[s2][:, 2 + g, :], Sb[s2][:, gs_], True, True, [t_bct[s2], t_Sb[s2]], [t_ps[6]])
            P.mm(psum[:, 7, :], xa[s2][:, 1024 + g * 128:1024 + (g + 1) * 128], xdtd[s2][:, gs_], True, True,
                 [t_xa[s2], t_xdtd[s2]], [t_ps[7]])
            Sg = S[:, gs_].rearrange("p (h d) -> p h d", h=8)
            P.tt(Sg, Sg, d_[:, 112 + g * 8:120 + g * 8].unsqueeze(2).to_broadcast([128, 8, 64]), ALU.mult, [t_S, td], [t_S])
            P.tt(S[:, gs_], S[:, gs_], psum[:, 7, :], ALU.add, [t_S, t_ps[7]], [t_S])
            P.copy(Sb[sn][:, gs_], S[:, gs_], [t_S], [t_Sb[sn]], eng="scalar")
            P.tt(y1[:, gs_].rearrange("p (h d) -> p h d", h=8), psum[:, 6, :].rearrange("p (h d) -> p h d", h=8),
                 ncs[s2][:, 16 + g * 8:24 + g * 8].unsqueeze(2).to_broadcast([128, 8, 64]), ALU.mult,
                 [t_ps[6], t_ncs[s2]], [t_y1])
            P.tt(y1[:, gs_], psum[:, 5, :], y1[:, gs_], ALU.add, [t_ps[5], t_y1], [t_y1])
        P.tt(y2[:, :], y1[:, :], sz[s2][:, :], ALU.mult, [t_y1, t_sz[s2]], [t_y2])
        P.act(scr_b[:, :], y2[:, :], AF.Square, [t_y2], [t_scrb, t_stb], accum_out=st_b[:, 0:1])
        rstd_from_ss(P, st_b, 0, 1, 1.0 / 1024, t_stb)
        P.stt(yn[s2][:, :], y2[:, :], st_b[:, 1:2], gssd[:, :], ALU.mult, ALU.mult, [t_y2, t_stb, t_row], [t_yn[s2]])
        P.dma("sync", yssd_dst[tok, :], yn[s2][:, :], "a_y%d" % s2, [t_yn[s2]], [])

    if NFILL > 0:
        P.fill = (NFILL, psum[:, 4, 64:512], K.identB, K.mneg[:, 0:448])
    run_pipeline(chunk, NCH, 2 if state_only else 4, DEP)
    P.fill = None
    if state_only:
        P.dma("sync", st_out[0:128, 0:1024], S[:, :], "st_st", [t_S], [])
        P.dma("gpsimd", st_out[128:131, :], raw[125:128, :], "st_st2", [t_raw], [])


def run_pipeline(body, n, offset, depth=2):
    active = []
    nxt = 0
    while nxt < n or active:
        if nxt < n and (not active or (len(active) < depth and active[-1][1] >= offset)):
            active.append([body(nxt), 0])
            nxt += 1
        for a in list(active):
            try:
                next(a[0])
                a[1] += 1
            except StopIteration:
                active.remove(a)


def mixer_B(C, K, L, T_tok, h_src, h_dst, hnT_src, yssd_src, w_in, sgu_g, sgu_b, wsT_ap, bspT_ap, sguo_g,
            w_out, ffn_g, hnT2_dst, w_router, gates_dst):
    P = C.P
    NCH = T_tok // 128
    tg = "B%d_" % L
    sb = C.sb
    moe = w_router is not None
    win = sb(tg + "win", [128, 8, 2048], BF16)
    wout = sb(tg + "wout", [128, 16, 1024], BF16)
    rows = sb(tg + "rows", [128, 4, 1024], F32)
    wsf = sb(tg + "wsf", [128, 8, 128], F32)
    wsT = sb(tg + "wsT", [128, 8, 128], BF16)
    bsp = sb(tg + "bsp", [128, 8], F32)
    t_win, t_wout, t_rows, t_ws = T("win"), T("wout"), T("rows"), T("ws")
    for q in range(4):
        P.dma("gpsimd", win[:, :, q * 512:(q + 1) * 512],
              w_in[:, 2576 + q * 512:2576 + (q + 1) * 512].rearrange("(k p) f -> p k f", p=128), "cw_b_w", [], [t_win])
    for q in range(2):
        P.dma("gpsimd", wout[:, :, q * 512:(q + 1) * 512],
              w_out[:, q * 512:(q + 1) * 512].rearrange("(k p) f -> p k f", p=128), "cw_b_o", [], [t_wout])
    for i, r in enumerate((sgu_g, sgu_b, sguo_g, ffn_g)):
        P.dma("sync", rows[:, i, :], r.partition_broadcast(128), "cr_b_r", [], [t_rows])
    P.dma("sync", wsf[:, :, :], wsT_ap, "cr_b", [], [t_ws])
    P.dma("sync", bsp[:, :], bspT_ap, "cr_b", [], [t_ws])
    P.tt(wsT[:, :, :], wsf[:, :, :], K.triU.unsqueeze(1).to_broadcast([128, 8, 128]), ALU.mult, [t_ws, K.t], [t_ws])
    if moe:
        wr = sb(tg + "wr", [128, 8, 8], F32)
        wrh = sb(tg + "wrh", [128, 8, 8], BF16)
        wrl = sb(tg + "wrl", [128, 8, 8], BF16)
        t_wr = T("wr")
        P.dma("sync", wr[:, :, :], w_router, "cr_b2", [], [t_wr])
        P.copy(wrh[:, :, :], wr[:, :, :], [t_wr], [t_wr])
        P.tt(wrl[:, :, :], wr[:, :, :], wrh[:, :, :], ALU.subtract, [t_wr], [t_wr])
    gsgu, bsgu, gsguo, gffn = rows[:, 0, :], rows[:, 1, :], rows[:, 2, :], rows[:, 3, :]

    DEP = 3

    def two(name, shape, dt):
        return [sb(tg + name + "%d" % s, shape, dt) for s in range(DEP)]

    def twoT(name):
        return [T(name + "%d" % s) for s in range(DEP)]

    hin = two("hin", [128, 1024], F32)
    hnT = two("hnT", [128, 8, 128], BF16)
    ycat = two("ycat", [128, 2048], BF16)
    u = two("u", [128, 1024], BF16)
    v = sb(tg + "v", [128, 1024], F32)
    vn = sb(tg + "vn", [128, 1024], F32)
    vnb = sb(tg + "vnb", [128, 1024], BF16)
    scr_a = sb(tg + "scra", [128, 1024], BF16)
    scr_b = sb(tg + "scrb", [128, 1024], BF16)
    st_l = sb(tg + "stl", [128, 16], F32)
    st_a = sb(tg + "sta", [128, 4], F32)
    st_b = sb(tg + "stb", [128, 4], F32)
    t1 = sb(tg + "t1", [128, 1024], F32)
    ycT = sb(tg + "ycT", [128, 16, 128], BF16)
    hnew = two("hnew", [128, 1024], F32)
    hn2 = sb(tg + "hn2", [128, 1024], F32)
    hn2T = two("hn2T", [128, 8, 128], BF16)
    t_hin, t_hnT, t_ycat, t_u, t_hnew, t_hn2T = [twoT(n) for n in ("hin", "hnT", "ycat", "u", "hnew", "hn2T")]
    t_v, t_vn, t_vnb, t_scra, t_scrb, t_stl, t_sta, t_stb, t_t1, t_ycT, t_hn2 = [T(n) for n in (
        "v", "vn", "vnb", "scra", "scrb", "stl", "sta", "stb", "t1", "ycT", "hn2")]
    if moe:
        hn2Tl = sb(tg + "hn2Tl", [128, 8, 128], BF16)
        lg = two("lg", [128, 48], F32)
        t_hn2Tf = T("hn2Tl")
        t_lg = twoT("lg")
        gall = sb(tg + "gall", [128, NCH, 8], F32)
        t_gall = T("gall")
    psum, t_ps = C.psum, C.t_ps
    ps_tr = psum[:, 0:2, :].rearrange("p a (k t) -> p (a k) t", k=4)

    def chunk(c):
        s2 = c % DEP
        tok = slice(c * 128, (c + 1) * 128)
        for cc in ([0, 1] if c == 0 else [c + 1]):
            if cc < NCH:
                sc_ = cc % DEP
                tk = slice(cc * 128, (cc + 1) * 128)
                P.dma("sync", hnT[sc_][:, :, :], hnT_src[:, :, tk].rearrange("k p t -> p k t"), "b_hnT%d" % sc_, [],
                      [t_hnT[sc_]])
                P.dma("sync", ycat[sc_][:, 0:1024], yssd_src[tk, :], "b_y%d" % sc_, [], [t_ycat[sc_]])
                P.dma("sync", hin[sc_][:, :], h_src[tk, :], "b_h%d" % sc_, [], [t_hin[sc_]])
        for q in range(4):
            pb = q % 2
            for k in range(8):
                P.mm(psum[:, pb, :], hnT[s2][:, k, :], win[:, k, q * 512:(q + 1) * 512], k == 0, k == 7,
                     [t_hnT[s2], t_win], [t_ps[pb]])
            if q < 2:
                P.act(u[s2][:, q * 512:(q + 1) * 512], psum[:, pb, :], AF.Gelu, [t_ps[pb]], [t_u[s2]])
            else:
                P.act(v[:, (q - 2) * 512:(q - 1) * 512], psum[:, pb, :], AF.Gelu, [t_ps[pb]], [t_v])
        yield
        for q in range(2):
            P.op("vector", lambda e, q=q: e.bn_stats(st_l[:, q * 6:6 + q * 6], v[:, q * 512:(q + 1) * 512]),
                 [t_v], [t_stl])
        P.op("vector", lambda e: e.bn_aggr(st_l[:, 12:14], st_l[:, 0:12]), [t_stl], [t_stl])
        rstd_ops(P, st_l[:, 14:15], st_l[:, 13:14], 1.0, [t_stl], [t_stl])
        P.ts(vn[:, :], v[:, :], st_l[:, 12:13], st_l[:, 14:15], ALU.subtract, ALU.mult, [t_v, t_stl], [t_vn])
        P.tt(vn[:, :], vn[:, :], gsgu, ALU.mult, [t_vn, t_rows], [t_vn], eng="gpsimd")
        P.tt(vnb[:, :], vn[:, :], bsgu, ALU.add, [t_vn, t_rows], [t_vnb], eng="gpsimd")
        yield
        for h in range(8):
            pb = 6 + h // 4
            P.mm(psum[:, pb, (h % 4) * 128:(h % 4 + 1) * 128], wsT[:, h, :], vnb[:, h * 128:(h + 1) * 128], True, True,
                 [t_ws, t_vnb], [t_ps[pb]])
        P.tt(t1[:, :].rearrange("p (h d) -> p h d", h=8), psum[:, 6:8, :].rearrange("p a (h d) -> p (a h) d", h=4),
             bsp[:, :].unsqueeze(2).to_broadcast([128, 8, 128]), ALU.add, [t_ps[6], t_ps[7], t_ws], [t_t1])
        P.tt(t1[:, :], t1[:, :], u[s2][:, :], ALU.mult, [t_t1, t_u[s2]], [t_t1])
        P.act(scr_a[:, :], t1[:, :], AF.Square, [t_t1], [t_scra, t_sta], accum_out=st_a[:, 0:1])
        rstd_from_ss(P, st_a, 0, 1, 1.0 / 1024, t_sta)
        P.stt(ycat[s2][:, 1024:2048], t1[:, :], st_a[:, 1:2], gsguo, ALU.mult, ALU.mult, [t_t1, t_sta, t_rows],
              [t_ycat[s2]])
        yield
        ps_y = psum[:, 2:4, :].rearrange("p a b -> p (a b)").bitcast(BF16).rearrange("p (k t) -> p k t", k=16)
        for k in range(16):
            P.tr(ps_y[:, k, :], ycat[s2][:, k * 128:(k + 1) * 128], K.identB, [t_ycat[s2], K.t], [t_ps[2 + k // 8]])
        P.copy(ycT[:, :, :], ps_y, [t_ps[2], t_ps[3]], [t_ycT], eng="scalar")
        yield
        for hd in range(2):
            for k in range(16):
                P.mm(psum[:, 4 + hd, :], ycT[:, k, :], wout[:, k, hd * 512:(hd + 1) * 512], k == 0, k == 15,
                     [t_ycT, t_wout], [t_ps[4 + hd]])
        P.tt(hnew[s2][:, :].rearrange("p (a b) -> p a b", a=2), psum[:, 4:6, :],
             hin[s2][:, :].rearrange("p (a b) -> p a b", a=2), ALU.add, [t_ps[4], t_ps[5], t_hin[s2]], [t_hnew[s2]])
        P.dma("sync", h_dst[tok, :], hnew[s2][:, :], "b_ho%d" % s2, [t_hnew[s2]], [])
        yield
        P.act(scr_b[:, :], hnew[s2][:, :], AF.Square, [t_hnew[s2]], [t_scrb, t_stb], accum_out=st_b[:, 0:1])
        rstd_from_ss(P, st_b, 0, 1, 1.0 / 1024, t_stb)
        P.stt(hn2[:, :], hnew[s2][:, :], st_b[:, 1:2], gffn, ALU.mult, ALU.mult, [t_hnew[s2], t_stb, t_rows], [t_hn2])
        for k in range(8):
            P.tr(ps_tr[:, k, :], hn2[:, k * 128:(k + 1) * 128], K.identF, [t_hn2, K.t], [t_ps[k // 4]])
        P.copy(hn2T[s2][:, :, :], ps_tr, [t_ps[0], t_ps[1]], [t_hn2T[s2]], eng="scalar")
        P.dma("scalar", hnT2_dst[:, :, tok].rearrange("k p t -> p k t"), hn2T[s2][:, :, :], "b_h2T%d" % s2,
              [t_hn2T[s2]], [])
        if moe:
            l_ = lg[s2]
            tl = t_lg[s2]
            P.tt(hn2Tl[:, :, :], ps_tr, hn2T[s2][:, :, :], ALU.subtract, [t_ps[0], t_ps[1], t_hn2T[s2]], [t_hn2Tf])
            for k in range(8):
                P.mm(psum[:, 4, 0:8], hn2T[s2][:, k, :], wrh[:, k, :], k == 0, False, [t_hn2T[s2], t_wr], [t_ps[4]])
                P.mm(psum[:, 4, 0:8], hn2T[s2][:, k, :], wrl[:, k, :], False, False, [t_hn2T[s2], t_wr], [t_ps[4]])
                P.mm(psum[:, 4, 0:8], hn2Tl[:, k, :], wrh[:, k, :], False, k == 7, [t_hn2Tf, t_wr], [t_ps[4]])
            P.copy(l_[:, 0:8], psum[:, 4, 0:8], [t_ps[4]], [tl])
            P.op("vector", lambda e, l_=l_: e.max(l_[:, 8:16], l_[:, 0:8]), [tl], [tl])
            P.ts(l_[:, 16:24], l_[:, 0:8], l_[:, 9:10], None, ALU.is_ge, None, [tl], [tl])
            P.ts(l_[:, 40:41], l_[:, 8:9], -1.0, None, ALU.mult, None, [tl], [tl])
            P.act(l_[:, 24:32], l_[:, 0:8], AF.Exp, [tl], [tl], bias=l_[:, 40:41])
            P.tt(l_[:, 24:32], l_[:, 24:32], l_[:, 16:24], ALU.mult, [tl], [tl])
            P.op("vector", lambda e, l_=l_: e.tensor_reduce(l_[:, 41:42], l_[:, 24:32], mybir.AxisListType.X, ALU.add),
                 [tl], [tl])
            P.op("vector", lambda e, l_=l_: e.reciprocal(l_[:, 42:43], l_[:, 41:42]), [tl], [tl])
            P.ts(gall[:, c, :], l_[:, 24:32], l_[:, 42:43], None, ALU.mult, None, [tl], [t_gall])

    run_pipeline(chunk, NCH, 2, DEP)
    if moe:
        P.dma("sync", gates_dst, gall[:, :, :], "b_g", [t_gall], [])


D_IN_PROJ = 4624
NOMOE = False
NFILL = 0
MOE_STEPS = 3
PAIRS = [[0, 1], [2, 3], [4, 5], [6, 7]]
WNAMES = [("norm_mix_g", [2, 1024]), ("w_in", [2, 1024, D_IN_PROJ]), ("conv_w", [2, 4, 1536]), ("conv_b", [2, 1536]),
          ("dt_bias", [2, 16]), ("a_log", [2, 16]), ("d_skip", [2, 16]), ("ssd_norm_g", [2, 1024]),
          ("sgu_norm_g", [2, 1024]), ("sgu_norm_b", [2, 1024]), ("wsT", [2, 128, 8, 128]), ("bspT", [2, 128, 8]),
          ("sgu_out_g", [2, 1024]), ("w_out", [2, 2048, 1024]), ("norm_ffn_g", [2, 1024]),
          ("ffn_w_gate", [1, 1024, D_FF]), ("ffn_w_up", [1, 1024, D_FF]), ("ffn_w_down", [1, D_FF, 1024]),
          ("wr_l", [128, 8, 8]), ("moe_w_gate", [1, 8, 1024, D_FF]), ("moe_w_up", [1, 8, 1024, D_FF]),
          ("moe_w_down", [1, 8, D_FF, 1024]), ("final_norm_g", [1024])]


def build_program(T_tok=4096, TH=2048, layers=(0, 1), n_exp=N_EXPERTS, d_ff=D_FF, stop_after=None, dbg=False):
    nc = bass.Bass("TRN2", target_bir_lowering=False)
    I = {}
    I["x"] = nc.dram_tensor("x", [T_tok, 1024], F32, kind="ExternalInput").ap()
    for n, shp in WNAMES:
        shp = list(shp)
        if n in ("ffn_w_gate", "ffn_w_up"):
            shp[2] = d_ff
        if n == "ffn_w_down":
            shp[1] = d_ff
        if n in ("moe_w_gate", "moe_w_up"):
            shp[3] = d_ff
        if n == "moe_w_down":
            shp[2] = d_ff
        I[n] = nc.dram_tensor(n, shp, F32, kind="ExternalInput").ap()
    I["flag"] = nc.dram_tensor("flag", [128, 1], F32, kind="ExternalInput").ap()
    I["cf"] = nc.dram_tensor("cf", [128, 384], F32, kind="ExternalInput").ap()
    I["cb"] = nc.dram_tensor("cb", [128, 1792], BF16, kind="ExternalInput").ap()
    out = nc.dram_tensor("out", [T_tok, 1024], F32, kind="ExternalOutput").ap()
    hA = nc.dram_tensor("hA", [T_tok, 1024], F32).ap()
    hB = nc.dram_tensor("hB", [T_tok, 1024], F32).ap()
    hnTa = nc.dram_tensor("hnTa", [8, 128, T_tok], BF16).ap()
    hnTb = nc.dram_tensor("hnTb", [8, 128, T_tok], BF16).ap()
    yssd = nc.dram_tensor("yssd", [T_tok, 1024], BF16).ap()
    gates = nc.dram_tensor("gates", [128, T_tok // 128, 8], F32).ap()
    cc_in = [nc.dram_tensor("cc_in%d" % L, [131, 1536], F32).ap() for L in range(2)]
    cc_out = [nc.dram_tensor("cc_out%d" % L, [2 * 131, 1536], F32).ap() for L in range(2)]
    with contextlib.ExitStack() as gs:
        P = Prog(nc, gs)
        C = Ctx(nc, gs, P)
        C.psum = gs.enter_context(nc.psum_tensor("psum", [128, 8, 512], F32))
        C.t_ps = [T("ps%d" % b) for b in range(8)]
        K = load_consts(C, I["cf"], I["cb"])
        C.t_const = K.t

        def phase(fn):
            with contextlib.ExitStack() as ps:
                C.stack = ps
                C.t_ps = [T("ps%d" % b) for b in range(8)]
                fn()
                P.emit()

        def done(tag, src):
            return stop_after == tag

        stopped = False
        for L in layers:
            if stopped:
                break
            src = I["x"] if L == layers[0] else hB
            a_args = (I["w_in"][L], I["norm_mix_g"][L], I["conv_w"][L], I["conv_b"][L], I["dt_bias"][L],
                      I["a_log"][L], I["d_skip"][L], I["ssd_norm_g"][L])
            phase(lambda: mixer_A(C, K, L, T_tok, src, *a_args, True, None, cc_in[L], I["flag"], None, None))
            P.op("gpsimd", lambda e, L=L: e.collective_compute("AllGather", ALU.bypass, replica_groups=PAIRS,
                                                              ins=[cc_in[L]], outs=[cc_out[L]]),
                 [], [], dma_key="cc", inc=1)
            P.emit()
            phase(lambda: mixer_A(C, K, L, T_tok, src, *a_args, False, cc_out[L], None, I["flag"], hnTa, yssd))
            moe = (L % 2 == 1) and not NOMOE
            dstB = hA
            phase(lambda: mixer_B(C, K, L, T_tok, src, dstB, hnTa, yssd, I["w_in"][L], I["sgu_norm_g"][L],
                                  I["sgu_norm_b"][L], I["wsT"][L], I["bspT"][L], I["sgu_out_g"][L], I["w_out"][L],
                                  I["norm_ffn_g"][L], hnTb, I["wr_l"] if moe else None,
                                  gates if moe else None))
            if stop_after == "mix%d" % L:
                fin_dst, stopped = hA, True
                break
            last = (L == layers[-1]) and (L == 1)
            if not moe:
                wl = [(I["ffn_w_gate"][0], I["ffn_w_up"][0], I["ffn_w_down"][0], None)]
                gsrc = None
            else:
                wl = [(I["moe_w_gate"][0, e], I["moe_w_up"][0, e], I["moe_w_down"][0, e], e) for e in range(n_exp)]
                gsrc = gates
            dstF = out if last else hB
            phase(lambda: ffn_phase(C, "F%d_" % L, T_tok, TH, hA, hnTb, gsrc, wl, dstF,
                                    fin_g=I["final_norm_g"] if last else None))
            if stop_after == "ffn%d" % L and not last:
                fin_dst, stopped = hB, True
                break
        if stopped:
            def cp():
                tmp = C.sb("dbgcp", [128, T_tok // 128, 1024], F32)
                tt_ = T("dbg")
                P.dma("sync", tmp[:, :, :], fin_dst.rearrange("(j p) d -> p j d", p=128), "dbg", [], [tt_])
                P.dma("sync", out.rearrange("(j p) d -> p j d", p=128), tmp[:, :, :], "dbg", [tt_], [])
            phase(cp)
    return nc


_NC_CACHE = {}


def make_in_maps(inputs, n_cores=8, T_tok=4096):
    cf, cb = host_consts()
    x = np.asarray(inputs["x"], dtype=np.float32)
    B, S, D = x.shape
    halves = S // T_tok
    shared = {}
    for n, _ in WNAMES:
        if n == "wsT":
            shared[n] = np.ascontiguousarray(np.asarray(inputs["w_spatial"], np.float32).transpose(0, 3, 1, 2))
        elif n == "wr_l":
            shared[n] = np.ascontiguousarray(
                np.asarray(inputs["moe_w_router"], np.float32)[0].reshape(8, 128, 8).transpose(1, 0, 2))
        elif n == "bspT":
            shared[n] = np.ascontiguousarray(np.asarray(inputs["b_spatial"], np.float32).transpose(0, 2, 1))
        else:
            shared[n] = np.ascontiguousarray(np.asarray(inputs[n], np.float32))
    shared["cf"] = cf
    shared["cb"] = cb
    maps = []
    for c in range(n_cores):
        b, hf = c // halves, c % halves
        m = dict(shared)
        m["x"] = np.ascontiguousarray(x[b, hf * T_tok:(hf + 1) * T_tok])
        m["flag"] = np.full((128, 1), float(hf), np.float32)
        maps.append(m)
    return maps


def kernel(**inputs):
    if "full" not in _NC_CACHE:
        _NC_CACHE["full"] = build_program()
    nc = _NC_CACHE["full"]
    maps = make_in_maps(inputs)
    res = run_bass_kernel_spmd(nc, maps, core_ids=list(range(8)))
    x = inputs["x"]
    B, S, D = x.shape
    outp = np.empty((B, S, D), np.float32)
    for c in range(8):
        b, hf = c // 2, c % 2
        outp[b, hf * 4096:(hf + 1) * 4096] = res.results[c]["out"]
    return outp
```

```python
import contextlib
import numpy as np
import concourse.bass as bass
import concourse.mybir as mybir
from concourse.bass_utils import run_bass_kernel_spmd

F32 = mybir.dt.float32
BF16 = mybir.dt.bfloat16
AF = mybir.ActivationFunctionType
ALU = mybir.AluOpType

D_MODEL = 1024
D_FF = 3584
N_EXPERTS = 8
ENGS = ("tensor", "vector", "scalar", "gpsimd", "sync")


class T:
    __slots__ = ("name", "last_w", "readers")

    def __init__(self, name):
        self.name = name
        self.last_w = None
        self.readers = {}


class Op:
    __slots__ = ("eng", "fn", "deps", "flag", "val", "sem", "is_dma", "key", "inc", "blk")


class Prog:
    def __init__(self, nc, gstack, same_eng_sync=True):
        self.nc = nc
        self.gstack = gstack
        self.base_waited = {}
        self.ops = {e: [] for e in ENGS}
        self.same_eng_sync = same_eng_sync
        self.dma_cnt = {}
        self.nops = 0
        self.blk = 0
        self.fill = None
        self.last_dma = {}

    def op(self, eng, fn, reads=(), writes=(), dma_key=None, inc=16):
        o = Op()
        o.inc = inc
        o.blk = self.blk
        o.eng = eng
        o.fn = fn
        o.flag = False
        o.val = 0
        o.sem = None
        o.is_dma = dma_key is not None
        o.key = dma_key
        deps = {}
        for t in reads:
            if t.last_w is not None:
                deps[id(t.last_w)] = t.last_w
        for t in writes:
            if t.last_w is not None:
                deps[id(t.last_w)] = t.last_w
            for r in t.readers.values():
                deps[id(r)] = r
        if dma_key is not None:
            prev = self.last_dma.get(dma_key)
            if prev is not None:
                deps[id(prev)] = prev
            self.last_dma[dma_key] = o
        dl = []
        for d in deps.values():
            if d is o or d.blk != self.blk:
                continue
            if (not d.is_dma) and d.eng == eng and not o.is_dma:
                if eng == "tensor" or not self.same_eng_sync:
                    continue
            dl.append(d)
        o.deps = dl
        if eng == "tensor" and self.fill is not None and dl and not getattr(self, "_in_fill", False):
            self._in_fill = True
            n, f_out, f_l, f_r = self.fill
            for _ in range(n):
                self.op("tensor", lambda e: e.matmul(f_out, f_l, f_r, start=True, stop=True), [], [])
            self._in_fill = False
        rk = (eng, dma_key)
        for t in reads:
            t.readers[rk] = o
        for t in writes:
            t.last_w = o
            t.readers = {}
        self.ops[eng].append(o)
        self.nops += 1
        return o

    def mm(self, out, lhsT, rhs, start, stop, reads, writes):
        return self.op("tensor", lambda e: e.matmul(out, lhsT, rhs, start=start, stop=stop), reads, writes)

    def tr(self, out, in_, ident, reads, writes):
        return self.op("tensor", lambda e: e.transpose(out, in_, ident), reads, writes)

    def act(self, out, in_, func, reads, writes, bias=None, scale=None, accum_out=None, eng="scalar"):
        kw = {}
        if bias is not None:
            kw["bias"] = bias
        if scale is not None:
            kw["scale"] = scale
        if accum_out is not None:
            kw["accum_out"] = accum_out
        return self.op(eng, lambda e: e.activation(out, in_, func, **kw), reads, writes)

    def tt(self, out, in0, in1, op, reads, writes, eng="vector"):
        return self.op(eng, lambda e: e.tensor_tensor(out, in0, in1, op), reads, writes)

    def ts(self, out, in0, s1, s2, op0, op1, reads, writes, eng="vector", accum_out=None):
        if op1 is None:
            return self.op(eng, lambda e: e.tensor_scalar(out, in0, s1, None, op0), reads, writes)
        if accum_out is not None:
            return self.op(eng, lambda e: e.tensor_scalar(out, in0, s1, s2, op0, op1, accum_out=accum_out), reads, writes)
        return self.op(eng, lambda e: e.tensor_scalar(out, in0, s1, s2, op0, op1), reads, writes)

    def stt(self, out, in0, scalar, in1, op0, op1, reads, writes):
        return self.op("vector", lambda e: e.scalar_tensor_tensor(out, in0, scalar, in1, op0, op1), reads, writes)

    def copy(self, out, in_, reads, writes, eng="vector"):
        if eng == "scalar":
            return self.act(out, in_, AF.Copy, reads, writes)
        return self.op(eng, lambda e: e.tensor_copy(out, in_), reads, writes)

    def dma(self, eng, out, in_, key, reads, writes, **kw):
        return self.op(eng, lambda e: e.dma_start(out=out, in_=in_, **kw), reads, writes, dma_key=key)

    def emit(self, stack=None):
        nc = self.nc
        gs = self.gstack
        if not hasattr(self, "sems"):
            self.sems = {e: gs.enter_context(nc.semaphore("s_" + e)) for e in ENGS}
            self.keysem = {}
            self.eng_cnt = {e: 0 for e in ENGS}
        sems, keysem = self.sems, self.keysem
        for e in ENGS:
            for o in self.ops[e]:
                for d in o.deps:
                    d.flag = True
        for e in ENGS:
            c = self.eng_cnt[e]
            for o in self.ops[e]:
                if o.is_dma:
                    if o.key not in keysem:
                        keysem[o.key] = gs.enter_context(nc.semaphore("d_" + o.key))
                        self.dma_cnt[o.key] = 0
                    self.dma_cnt[o.key] += o.inc
                    o.val = self.dma_cnt[o.key]
                    o.sem = keysem[o.key]
                elif o.flag:
                    c += 1
                    o.val = c
                    o.sem = sems[e]
            self.eng_cnt[e] = c
        prog = self
        base_waited = dict(self.base_waited)

        def body(ename):
            def _f(eng):
                waited = dict(base_waited)
                for o in prog.ops[ename]:
                    need = {}
                    for d in o.deps:
                        k = id(d.sem)
                        if k not in need or need[k][1] < d.val:
                            need[k] = (d.sem, d.val)
                    for k, (s, v) in need.items():
                        if waited.get(k, 0) < v:
                            eng.wait_ge(s, v)
                            waited[k] = v
                    inst = o.fn(eng)
                    if o.is_dma:
                        if o.inc == 16:
                            inst.then_inc(o.sem, 16)
                        else:
                            inst.then_inc(o.sem)
                    elif o.flag:
                        inst.then_inc(o.sem, 1)
                if ename == "sync":
                    for k, s in keysem.items():
                        eng.wait_ge(s, prog.dma_cnt[k])
            return _f

        with nc.Block() as block:
            block.tensor(body("tensor"))
            block.vector(body("vector"))
            block.scalar(body("scalar"))
            block.gpsimd(body("gpsimd"))
            block.sync(body("sync"))
        for e in ENGS:
            self.base_waited[id(sems[e])] = self.eng_cnt[e]
        for k, sm in keysem.items():
            self.base_waited[id(sm)] = self.dma_cnt[k]
        self.ops = {e: [] for e in ENGS}
        self.blk += 1


class Ctx:
    def __init__(self, nc, stack, prog):
        self.nc = nc
        self.stack = stack
        self.P = prog
        self._n = 0

    def sb(self, name, shape, dtype):
        return self.stack.enter_context(self.nc.sbuf_tensor(name, list(shape), dtype))

    def ps(self, name, shape, dtype):
        return self.stack.enter_context(self.nc.psum_tensor(name, list(shape), dtype))

    def dram(self, name, shape, dtype, kind="Internal"):
        return self.nc.dram_tensor(name, list(shape), dtype, kind=kind).ap()


def ffn_phase(C, tag, T_tok, TH, h_src, hnT_src, gates_src, wlist, h_dst, fin_g=None,
              h_src_tiles=None, hnT_tiles=None, gates_tiles=None, h_dst_tiles=None, FC=512, emit_every=2):
    P = C.P
    NJ = TH // 128
    NTT = TH // 512
    NFS = FC // 128
    acc = C.sb(tag + "acc", [128, NJ, 1024], F32)
    hnT = C.sb(tag + "hnT", [128, 8, TH], BF16)
    wg = [C.sb(tag + "wg%d" % s, [128, 8, FC], BF16) for s in range(2)]
    wu = [C.sb(tag + "wu%d" % s, [128, 8, FC], BF16) for s in range(2)]
    wd = [C.sb(tag + "wd%d" % s, [128, NFS, 1024], BF16) for s in range(2)]
    actb = [C.sb(tag + "act%d" % s, [128, NFS, 512], BF16) for s in range(2)]
    sgb = [C.sb(tag + "sg%d" % s, [128, 512], F32) for s in range(2)]
    gts = C.sb(tag + "gts", [128, NJ, 8], F32) if gates_src is not None else None
    psum = C.psum
    t_acc = [T("acc%d" % j) for j in range(NJ)]
    t_hnT = [T("hnT%d" % j) for j in range(NTT)]
    t_wg = [T("wg%d" % s) for s in range(2)]
    t_wu = [T("wu%d" % s) for s in range(2)]
    t_wd = [T("wd%d" % s) for s in range(2)]
    t_act = [T("act%d" % s) for s in range(2)]
    t_sg = [T("sg%d" % s) for s in range(2)]
    t_gts = T("gts")
    t_ps = C.t_ps
    if fin_g is not None:
        fgrow = C.sb(tag + "fgrow", [128, 1024], F32)
        t_fg = T("fgrow")
        P.dma("sync", fgrow[:, :], fin_g.partition_broadcast(128), "ffn_fg", [], [t_fg])
        fscr = C.sb(tag + "fscr", [128, 1024], BF16)
        fss = C.sb(tag + "fss", [128, 2], F32)
        fout = [C.sb(tag + "fout%d" % s, [128, 1024], F32) for s in range(2)]
        t_fscr, t_fss = T("fscr"), T("fss")
        t_fout = [T("fout%d" % s) for s in range(2)]

    nhalf = T_tok // TH
    wci = 0
    for half in range(nhalf):
        t0 = half * TH
        for j in range(NJ):
            rd = [h_src_tiles[(t0 // 128) + j]] if h_src_tiles else []
            P.dma("sync", acc[:, j, :], h_src[t0 + j * 128: t0 + (j + 1) * 128, :], "ffn_acc%d" % j,
                  rd, [t_acc[j]])
        for tt in range(NTT):
            rd = [hnT_tiles[(t0 // 128) + tt * 4 + q] for q in range(4)] if hnT_tiles else []
            P.dma("sync", hnT[:, :, tt * 512:(tt + 1) * 512],
                  hnT_src[:, :, t0 + tt * 512: t0 + (tt + 1) * 512].rearrange("k p t -> p k t"),
                  "ffn_hnT%d" % tt, rd, [t_hnT[tt]])
        if gts is not None:
            rd = [gates_tiles[(t0 // 128) + j] for j in range(NJ)] if gates_tiles else []
            P.dma("sync", gts[:, :, :], gates_src[:, t0 // 128:(t0 + TH) // 128, :],
                  "ffn_gts", rd, [t_gts])

        pend = None

        def down(slot, tt, gi, aslot):
            for st in range(4):
                j = tt * 4 + st
                pb = 4 + 2 * (j % 2)
                for hd in range(2):
                    for fs in range(NFS):
                        P.mm(psum[:, pb + hd, :], actb[aslot][:, fs, st * 128:(st + 1) * 128],
                             wd[slot][:, fs, hd * 512:(hd + 1) * 512], fs == 0, fs == NFS - 1,
                             [t_act[aslot], t_wd[slot]], [t_ps[pb + hd]])
                po = psum[:, pb:pb + 2, :]
                av = acc[:, j, :].rearrange("p (a b) -> p a b", a=2)
                if gi is None:
                    P.tt(av, po, av, ALU.add, [t_ps[pb], t_ps[pb + 1], t_acc[j]], [t_acc[j]])
                else:
                    P.stt(av, po, gts[:, j, gi:gi + 1], av, ALU.mult, ALU.add,
                          [t_ps[pb], t_ps[pb + 1], t_acc[j], t_gts], [t_acc[j]])

        tti = 0
        for wi, (Wg, Wu, Wd, gi) in enumerate(wlist):
            if wi > 0 and emit_every and wi % emit_every == 0:
                down(*pend)
                pend = None
                P.emit()
            Fdim = Wg.shape[1]
            for fc in range(Fdim // FC):
                slot = wci % 2
                wci += 1
                P.dma("gpsimd", wg[slot][:, :, :],
                      Wg[:, fc * FC:(fc + 1) * FC].rearrange("(k p) f -> p k f", p=128),
                      "w_g%d" % slot, [], [t_wg[slot]])
                P.dma("gpsimd", wu[slot][:, :, :],
                      Wu[:, fc * FC:(fc + 1) * FC].rearrange("(k p) f -> p k f", p=128),
                      "w_u%d" % slot, [], [t_wu[slot]])
                P.dma("gpsimd", wd[slot][:, :, :],
                      Wd[fc * FC:(fc + 1) * FC, :].rearrange("(s p) d -> p s d", p=128),
                      "w_d%d" % slot, [], [t_wd[slot]])
                for tt in range(NTT):
                    aslot = tti % 2
                    tti += 1
                    for fs in range(NFS):
                        b = fs % 2
                        pg, pu = psum[:, b, :], psum[:, 2 + b, :]
                        for kc in range(8):
                            P.mm(pg, wg[slot][:, kc, fs * 128:(fs + 1) * 128], hnT[:, kc, tt * 512:(tt + 1) * 512],
                                 kc == 0, kc == 7, [t_wg[slot], t_hnT[tt]], [t_ps[b]])
                        for kc in range(8):
                            P.mm(pu, wu[slot][:, kc, fs * 128:(fs + 1) * 128], hnT[:, kc, tt * 512:(tt + 1) * 512],
                                 kc == 0, kc == 7, [t_wu[slot], t_hnT[tt]], [t_ps[2 + b]])
                        P.act(sgb[b][:, :], pg, AF.Silu, [t_ps[b]], [t_sg[b]])
                        P.tt(actb[aslot][:, fs, :], pu, sgb[b][:, :], ALU.mult,
                             [t_ps[2 + b], t_sg[b]], [t_act[aslot]])
                    if pend is not None:
                        down(*pend)
                    pend = (slot, tt, gi, aslot)
        down(*pend)
        pend = None
        for j in range(NJ):
            jj = (t0 // 128) + j
            if fin_g is None:
                wr = [h_dst_tiles[jj]] if h_dst_tiles else []
                P.dma("sync", h_dst[t0 + j * 128: t0 + (j + 1) * 128, :], acc[:, j, :], "ffn_st%d" % (j % 2),
                      [t_acc[j]], wr)
            else:
                s = j % 2
                P.act(fscr[:, :], acc[:, j, :], AF.Square, [t_acc[j]], [t_fscr, t_fss], accum_out=fss[:, 0:1])
                rstd_ops(P, fss[:, 1:2], fss[:, 0:1], 1.0 / 1024, [t_fss], [t_fss])
                P.stt(fout[s][:, :], acc[:, j, :], fss[:, 1:2], fgrow[:, :], ALU.mult, ALU.mult,
                      [t_acc[j], t_fss, t_fg], [t_fout[s]])
                P.dma("sync", h_dst[t0 + j * 128: t0 + (j + 1) * 128, :], fout[s][:, :], "ffn_st%d" % s,
                      [t_fout[s]], [])


def rstd_ops(P, out, ss, inv_n, reads, writes, eps=1e-6):
    P.ts(out, ss, inv_n, eps, ALU.mult, ALU.add, reads, writes)
    P.act(out, out, AF.Sqrt, reads, writes)
    P.op("vector", lambda e: e.reciprocal(out, out), reads, writes)


def host_consts():
    import ml_dtypes
    i = np.arange(128)
    ident = np.eye(128, dtype=np.float32)
    triU = (i[:, None] <= i[None, :]).astype(np.float32)
    ones = np.ones((128, 128), np.float32)
    cf = np.concatenate([ident, triU, ones], axis=1)
    sc = [(i[:, None] == (i[None, :] - (3 - k))).astype(np.float32) for k in range(3)]
    sp = [(i[:, None] == (128 + i[None, :] - (3 - k))).astype(np.float32) for k in range(3)]
    mneg = np.where(i[None, :] < i[:, None], -30000.0, 0.0).astype(np.float32)
    mneg4 = np.tile(mneg, (1, 4))
    onesrow = np.zeros((128, 128), np.float32)
    onesrow[0, :] = 1.0
    cb = np.concatenate([ident] + sc + sp + [mneg4, onesrow, ones, triU], axis=1).astype(ml_dtypes.bfloat16)
    return cf, cb


class Consts:
    pass


def load_consts(C, cf_ap, cb_ap):
    P = C.P
    K = Consts()
    gs = C.P.gstack
    cf = gs.enter_context(C.nc.sbuf_tensor("cst_f", [128, 384], F32))
    cb = gs.enter_context(C.nc.sbuf_tensor("cst_b", [128, 1792], BF16))
    K.t = T("consts")
    P.dma("sync", cf[:, :], cf_ap, "const", [], [K.t])
    P.dma("sync", cb[:, :], cb_ap, "const", [], [K.t])
    K.identF = cf[:, 0:128]
    K.triU = cf[:, 128:256]
    K.onesF = cf[:, 256:384]
    K.identB = cb[:, 0:128]
    K.Sc = [cb[:, 128 * (1 + k):128 * (2 + k)] for k in range(3)]
    K.Sp = [cb[:, 128 * (4 + k):128 * (5 + k)] for k in range(3)]
    K.mneg = cb[:, 896:1408]
    K.onesrow = cb[0:1, 1408:1536]
    K.onesB = cb[:, 1536:1664]
    K.triUB = cb[:, 1664:1792]
    return K


def rstd_act(P, st, i_ss, i_out, inv_n, tl, eps=1e-6):
    o_, i_ = st[:, i_out:i_out + 1], st[:, i_ss:i_ss + 1]
    P.act(o_, i_, AF.Ln, [tl], [tl], bias=eps, scale=inv_n)
    P.act(o_, o_, AF.Exp, [tl], [tl], scale=-0.5)


def rstd_from_ss(P, st, i_ss, i_out, inv_n, tl, eps=1e-6):
    rstd_ops(P, st[:, i_out:i_out + 1], st[:, i_ss:i_ss + 1], inv_n, [tl], [tl], eps)


def mixer_A(C, K, L, T_tok, h_src, w_in, norm_g, conv_w, conv_b, dt_bias, a_log, d_skip, ssd_g,
            state_only, st_in, st_out, flag_ap, hnT_dst, yssd_dst):
    P = C.P
    NCH = T_tok // 128
    tg = "A%d%s_" % (L, "p" if state_only else "m")
    sb = C.sb
    NW = 2576
    win = sb(tg + "win", [128, 8, NW], BF16)
    gmix = sb(tg + "gmix", [128, 1024], F32)
    gssd = sb(tg + "gssd", [128, 1024], F32)
    cw = sb(tg + "cw", [128, 4, 1536], BF16)
    cbias = sb(tg + "cbias", [1, 1536], BF16)
    sm = sb(tg + "sm", [128, 64], F32)
    t_win, t_row, t_sm = T("win"), T("rows"), T("sm")
    blocks = [(0, 512), (512, 512), (1024, 512), (1536, 512), (2048, 512), (2560, 16)]
    for (c0, cn) in blocks:
        P.dma("gpsimd", win[:, :, c0:c0 + cn], w_in[:, c0:c0 + cn].rearrange("(k p) f -> p k f", p=128),
              "cw_a_w", [], [t_win])
    P.dma("sync", gmix[:, :], norm_g.partition_broadcast(128), "cr_a", [], [t_row])
    P.dma("sync", gssd[:, :], ssd_g.partition_broadcast(128), "cr_a", [], [t_row])
    for k in range(4):
        P.dma("gpsimd", cw[:, k, :], conv_w[k, :].partition_broadcast(128), "cw_a", [], [t_row])
    P.dma("gpsimd", cbias[:, :], conv_b.unsqueeze(0), "cw_a", [], [t_row])
    P.dma("sync", sm[:, 0:16], dt_bias.partition_broadcast(128), "cr_a", [], [t_sm])
    P.dma("sync", sm[:, 16:32], a_log.partition_broadcast(128), "cr_a", [], [t_sm])
    P.dma("sync", sm[:, 32:48], d_skip.partition_broadcast(128), "cr_a", [], [t_sm])
    P.dma("sync", sm[:, 48:49], flag_ap, "cr_a", [], [t_sm])
    P.act(sm[:, 16:32], sm[:, 16:32], AF.Exp, [t_sm], [t_sm])
    P.ts(sm[:, 16:32], sm[:, 16:32], -1.0, None, ALU.mult, None, [t_sm], [t_sm])
    dtb, arow, dsk, flg = sm[:, 0:16], sm[:, 16:32], sm[:, 32:48], sm[:, 48:49]

    DEP = 3 if state_only else 2

    def two(name, shape, dt):
        return [sb(tg + name + "%d" % s, shape, dt) for s in range(DEP)]

    def twoT(name):
        return [T(name + "%d" % s) for s in range(DEP)]

    hin = two("hin", [128, 1024], F32)
    scr_a = sb(tg + "scra", [128, 1024], BF16)
    st_a = sb(tg + "sta", [128, 4], F32)
    hn = sb(tg + "hn", [128, 1024], F32)
    hnT = two("hnT", [128, 8, 128], BF16)
    raw = sb(tg + "raw", [128, 1536], BF16)
    rw = two("rw", [128, 4, 1536], BF16)
    xa = two("xa", [128, 1536], BF16)
    dts = two("dts", [128, 128], F32)
    xdtd = two("xdtd", [128, 1024], BF16)
    S = sb(tg + "S", [128, 1024], F32)
    t_hin, t_hnT, t_rw, t_xa, t_dts, t_xdtd = [twoT(n) for n in ("hin", "hnT", "rw", "xa", "dts", "xdtd")]
    t_scra, t_sta, t_hn, t_raw, t_S = [T(n) for n in ("scra", "sta", "hn", "raw", "S")]
    t_ps = C.t_ps
    psum = C.psum
    if not state_only:
        scr_b = sb(tg + "scrb", [128, 1024], BF16)
        st_b = sb(tg + "stb", [128, 4], F32)
        sz = two("sz", [128, 1024], BF16)
        ncs = two("ncs", [128, 32], F32)
        rhs1 = sb(tg + "rhs1", [128, 2, 16, 128], BF16)
        dab = sb(tg + "dab", [128, 32], BF16)
        bct = two("bct", [128, 4, 128], BF16)
        cbT = two("cbT", [128, 2, 128], BF16)
        decT = two("decT", [128, 16, 128], BF16)
        scT = two("scT", [128, 16, 128], BF16)
        xdt = two("xdt", [128, 1024], BF16)
        xds = two("xds", [128, 1024], BF16)
        Sb = two("Sb", [128, 1024], BF16)
        y1 = sb(tg + "y1", [128, 1024], F32)
        y2 = sb(tg + "y2", [128, 1024], F32)
        yn = two("yn", [128, 1024], BF16)
        t_sz, t_ncs, t_bct, t_cbT, t_decT, t_scT, t_xdt, t_xds, t_Sb, t_yn = [twoT(n) for n in (
            "sz", "ncs", "bct", "cbT", "decT", "scT", "xdt", "xds", "Sb", "yn")]
        t_scrb, t_stb, t_rhs1, t_y1, t_y2 = [T(n) for n in ("scrb", "stb", "rhs1", "y1", "y2")]

    if state_only:
        P.op("gpsimd", lambda e: e.memset(S[:, :], 0.0), [], [t_S])
        P.op("gpsimd", lambda e: e.memset(rw[DEP - 1][:, :, :], 0.0), [], [t_rw[DEP - 1]])
    else:
        P.dma("sync", S[:, :], st_in[0:128, 0:1024], "st_ld", [], [t_S])
        P.ts(S[:, :], S[:, :], flg, None, ALU.mult, None, [t_S, t_sm], [t_S])
        P.copy(Sb[0][:, :], S[:, :], [t_S], [t_Sb[0]], eng="scalar")
        rawh = y1
        P.op("gpsimd", lambda e: e.memset(rawh[:, :], 0.0), [], [t_y1])
        P.dma("sync", rawh[125:128, 0:768], st_in[128:131, 0:768], "st_ld2", [t_y1], [t_y1])
        P.ts(rawh[:, 0:768], rawh[:, 0:768], flg, None, ALU.mult, None, [t_y1, t_sm], [t_y1])
        for k in range(3):
            P.tt(rw[DEP - 1][:, k, 0:768], rawh[:, 0:768], cw[:, k, 0:768], ALU.mult, [t_y1, t_row], [t_rw[DEP - 1]],
                 eng="gpsimd")
        P.op("gpsimd", lambda e: e.memset(rawh[:, :], 0.0), [t_y1], [t_y1]) if False else None
        P.dma("sync", rawh[125:128, 0:768], st_in[128:131, 768:1536], "st_ld2", [t_y1, t_rw[DEP - 1]], [t_y1])
        P.ts(rawh[:, 0:768], rawh[:, 0:768], flg, None, ALU.mult, None, [t_y1, t_sm], [t_y1])
        for k in range(3):
            P.tt(rw[DEP - 1][:, k, 768:1536], rawh[:, 0:768], cw[:, k, 768:1536], ALU.mult, [t_y1, t_row],
                 [t_rw[DEP - 1]], eng="gpsimd")

    ps_hn = psum[:, 0:2, :].rearrange("p a (k t) -> p (a k) t", k=4)
    h16 = lambda ap: ap.rearrange("p (h d) -> p h d", h=16)
    pbs = [2, 3]

    def chunk(c):
        s2 = c % DEP
        sp = (c - 1) % DEP
        sn = (c + 1) % DEP
        tok = slice(c * 128, (c + 1) * 128)
        d_ = dts[s2]
        td = t_dts[s2]
        for cc in ([0, 1] if c == 0 else [c + 1]):
            if cc < NCH:
                P.dma("sync", hin[cc % DEP][:, :], h_src[cc * 128:(cc + 1) * 128, :], "a_h%d" % (cc % DEP), [],
                      [t_hin[cc % DEP]])
        P.act(scr_a[:, :], hin[s2][:, :], AF.Square, [t_hin[s2]], [t_scra, t_sta], accum_out=st_a[:, 0:1])
        rstd_act(P, st_a, 0, 1, 1.0 / 1024, t_sta)
        P.stt(hn[:, :], hin[s2][:, :], st_a[:, 1:2], gmix[:, :], ALU.mult, ALU.mult,
              [t_hin[s2], t_sta, t_row], [t_hn])
        yield
        for k in range(8):
            P.tr(ps_hn[:, k, :], hn[:, k * 128:(k + 1) * 128], K.identF, [t_hn, K.t], [t_ps[k // 4]])
        P.copy(hnT[s2][:, :, :], ps_hn, [t_ps[0], t_ps[1]], [t_hnT[s2]], eng="scalar")
        if not state_only:
            P.dma("scalar", hnT_dst[:, :, tok].rearrange("k p t -> p k t"), hnT[s2][:, :, :], "a_hnT%d" % s2,
                  [t_hnT[s2]], [])
        bi = 0
        for (c0, cn) in blocks:
            if c0 >= 1024 or state_only:
                continue
            pb = pbs[bi % 2]
            bi += 1
            for k in range(8):
                P.mm(psum[:, pb, 0:cn], hnT[s2][:, k, :], win[:, k, c0:c0 + cn], k == 0, k == 7,
                     [t_hnT[s2], t_win], [t_ps[pb]])
            P.act(sz[s2][:, c0:c0 + cn], psum[:, pb, 0:cn], AF.Silu, [t_ps[pb]], [t_sz[s2]])
        yield
        lite = state_only and c != NCH - 1
        for (c0, cn) in blocks:
            if c0 < 1024:
                continue
            if lite and c0 == 2048:
                cn = 256
            pb = pbs[bi % 2]
            bi += 1
            for k in range(8):
                P.mm(psum[:, pb, 0:cn], hnT[s2][:, k, :], win[:, k, c0:c0 + cn], k == 0, k == 7,
                     [t_hnT[s2], t_win], [t_ps[pb]])
            if c0 < 2560:
                P.copy(raw[:, c0 - 1024:c0 - 1024 + cn], psum[:, pb, 0:cn], [t_ps[pb]], [t_raw], eng="scalar")
            else:
                P.tt(d_[:, 0:16], psum[:, pb, 0:16], dtb, ALU.add, [t_ps[pb], t_sm], [td])
        P.act(d_[:, 16:32], d_[:, 0:16], AF.Exp, [td], [td])
        P.act(d_[:, 32:48], d_[:, 16:32], AF.Ln, [td], [td], bias=1.0)
        P.tt(d_[:, 48:64], d_[:, 32:48], arow, ALU.mult, [td, t_sm], [td])
        ncv = 1280 if lite else 1536
        for k in range(4):
            P.tt(rw[s2][:, k, 0:ncv], raw[:, 0:ncv], cw[:, k, 0:ncv], ALU.mult, [t_raw, t_row], [t_rw[s2]])
        yield
        for b3 in range(3):
            pb = pbs[b3 % 2]
            wd_ = 256 if (state_only and b3 == 2) else 512
            cs_ = slice(b3 * 512, b3 * 512 + wd_)
            po_ = psum[:, pb, 0:wd_]
            P.mm(po_, K.onesrow, cbias[0:1, cs_], True, False, [K.t, t_row], [t_ps[pb]])
            for k in range(3):
                P.mm(po_, K.Sp[k], rw[sp][:, k, cs_], False, False, [K.t, t_rw[sp]], [t_ps[pb]])
            for k in range(3):
                P.mm(po_, K.Sc[k], rw[s2][:, k, cs_], False, False, [K.t, t_rw[s2]], [t_ps[pb]])
            P.mm(po_, K.identB, rw[s2][:, 3, cs_], False, True, [K.t, t_rw[s2]], [t_ps[pb]])
            P.act(xa[s2][:, cs_], po_, AF.Silu, [t_ps[pb]], [t_xa[s2]])
        P.mm(psum[:, 4, 0:16], K.triU, d_[:, 48:64], True, True, [K.t, td], [t_ps[4]])
        P.mm(psum[:, 4, 16:32], K.onesF, d_[:, 48:64], True, True, [K.t, td], [t_ps[4]])
        P.copy(d_[:, 64:96], psum[:, 4, 0:32], [t_ps[4]], [td])
        P.tt(d_[:, 96:112], d_[:, 80:96], d_[:, 64:80], ALU.subtract, [td], [td])
        P.act(d_[:, 96:112], d_[:, 96:112], AF.Exp, [td], [td])
        P.act(d_[:, 112:128], d_[:, 80:96], AF.Exp, [td], [td])
        yield
        x3 = h16(xa[s2][:, 0:1024])
        dt_b = d_[:, 32:48].unsqueeze(2).to_broadcast([128, 16, 64])
        dte_b = d_[:, 96:112].unsqueeze(2).to_broadcast([128, 16, 64])
        ebl_b = d_[:, 112:128].unsqueeze(2).to_broadcast([128, 16, 64])
        if state_only:
            P.tt(h16(xdtd[s2][:, :]), x3, dt_b, ALU.mult, [t_xa[s2], td], [t_xdtd[s2]], eng="gpsimd")
            P.tt(h16(xdtd[s2][:, :]), h16(xdtd[s2][:, :]), dte_b, ALU.mult, [t_xdtd[s2], td], [t_xdtd[s2]], eng="gpsimd")
        else:
            P.tt(h16(xdt[s2][:, :]), x3, dt_b, ALU.mult, [t_xa[s2], td], [t_xdt[s2]], eng="gpsimd")
            P.tt(h16(xdtd[s2][:, :]), h16(xdt[s2][:, :]), dte_b, ALU.mult, [t_xdt[s2], td], [t_xdtd[s2]], eng="gpsimd")
            P.tt(h16(xds[s2][:, :]), x3, dsk.unsqueeze(2).to_broadcast([128, 16, 64]), ALU.mult,
                 [t_xa[s2], t_sm], [t_xds[s2]], eng="gpsimd")
            n_ = ncs[s2]
            P.ts(n_[:, 0:16], d_[:, 64:80], -1.0, None, ALU.mult, None, [td], [t_ncs[s2]])
            P.act(n_[:, 16:32], d_[:, 64:80], AF.Exp, [td], [t_ncs[s2]])
            ps_t = psum[:, 7, 256:512].bitcast(BF16).rearrange("p (a t) -> p a t", a=4)
            for q in range(4):
                P.tr(ps_t[:, q, :], xa[s2][:, 1024 + q * 128:1024 + (q + 1) * 128], K.identB, [t_xa[s2], K.t], [t_ps[7]])
            P.copy(bct[s2][:, :, :], ps_t, [t_ps[7]], [t_bct[s2]])
            for g in range(2):
                P.mm(psum[:, 7, g * 128:(g + 1) * 128], bct[s2][:, g, :], bct[s2][:, 2 + g, :], True, True,
                     [t_bct[s2]], [t_ps[7]])
            P.copy(cbT[s2][:, :, :], psum[:, 7, 0:256].rearrange("p (g l) -> p g l", g=2), [t_ps[7]], [t_cbT[s2]],
                   eng="scalar")
            P.copy(dab[:, 0:16], d_[:, 48:64], [td], [t_rhs1])
            P.tt(dab[:, 16:32], d_[:, 48:64], dab[:, 0:16], ALU.subtract, [td, t_rhs1], [t_rhs1])
            for hl in range(2):
                P.tt(rhs1[:, hl, :, :], K.triUB.unsqueeze(1).to_broadcast([128, 16, 128]),
                     dab[:, hl * 16:(hl + 1) * 16].unsqueeze(2).to_broadcast([128, 16, 128]), ALU.mult,
                     [K.t, t_rhs1], [t_rhs1])
            yield
            for hb in range(2):
                for q in range(2):
                    h0 = hb * 8 + q * 4
                    P.mm(psum[:, 5 + q, :], K.onesB, rhs1[:, 0, h0:h0 + 4, :].rearrange("p h l -> p (h l)"), True, False,
                         [K.t, t_rhs1], [t_ps[5 + q]])
                    P.mm(psum[:, 5 + q, :], K.onesB, rhs1[:, 1, h0:h0 + 4, :].rearrange("p h l -> p (h l)"), False, False,
                         [K.t, t_rhs1], [t_ps[5 + q]])
                    P.mm(psum[:, 5 + q, :], K.identB, K.mneg, False, True, [K.t], [t_ps[5 + q]])
                    for hh in range(4):
                        h = h0 + hh
                        P.act(decT[s2][:, h, :], psum[:, 5 + q, hh * 128:(hh + 1) * 128], AF.Exp,
                              [t_ps[5 + q], t_ncs[s2]], [t_decT[s2]], bias=n_[:, h:h + 1])
                P.tt(scT[s2][:, hb * 8:(hb + 1) * 8, :], decT[s2][:, hb * 8:(hb + 1) * 8, :],
                     cbT[s2][:, hb, :].unsqueeze(1).to_broadcast([128, 8, 128]), ALU.mult,
                     [t_decT[s2], t_cbT[s2]], [t_scT[s2]])
                yield
        S3 = h16(S[:, :])
        if state_only:
            for g in range(2):
                P.mm(psum[:, 5 + g, :], xa[s2][:, 1024 + g * 128:1024 + (g + 1) * 128], xdtd[s2][:, g * 512:(g + 1) * 512],
                     True, True, [t_xa[s2], t_xdtd[s2]], [t_ps[5 + g]])
            P.tt(S3, S3, ebl_b, ALU.mult, [t_S, td], [t_S])
            P.tt(S[:, :].rearrange("p (a b) -> p a b", a=2), S[:, :].rearrange("p (a b) -> p a b", a=2), psum[:, 5:7, :],
                 ALU.add, [t_S, t_ps[5], t_ps[6]], [t_S])
            return
        e_b = ncs[s2][:, 16:32].unsqueeze(2).to_broadcast([128, 16, 64])
        for g in range(2):
            gs_ = slice(g * 512, (g + 1) * 512)
            P.mm(psum[:, 5, :], K.identB, xds[s2][:, gs_], True, False, [K.t, t_xds[s2]], [t_ps[5]])
            for hh in range(8):
                h = g * 8 + hh
                P.mm(psum[:, 5, hh * 64:(hh + 1) * 64], scT[s2][:, h, :], xdt[s2][:, h * 64:(h + 1) * 64], False, hh == 7,
                     [t_scT[s2], t_xdt[s2]], [t_ps[5]])
            P.mm(psum[:, 6, :], bct[s2][:, 2 + g, :], Sb[s2][:, gs_], True, True, [t_bct[s2], t_Sb[s2]], [t_ps[6]])
            P.mm(psum[:, 7, :], xa[s2][:, 1024 + g * 128:1024 + (g + 1) * 128], xdtd[s2][:, gs_], True, True,
                 [t_xa[s2], t_xdtd[s2]], [t_ps[7]])
            Sg = S[:, gs_].rearrange("p (h d) -> p h d", h=8)
            P.tt(Sg, Sg, d_[:, 112 + g * 8:120 + g * 8].unsqueeze(2).to_broadcast([128, 8, 64]), ALU.mult, [t_S, td], [t_S])
            P.tt(S[:, gs_], S[:, gs_], psum[:, 7, :], ALU.add, [t_S, t_ps[7]], [t_S])
            P.copy(Sb[sn][:, gs_], S[:, gs_], [t_S], [t_Sb[sn]], eng="scalar")
            P.tt(y1[:, gs_].rearrange("p (h d) -> p h d", h=8), psum[:, 6, :].rearrange("p (h d) -> p h d", h=8),
                 ncs[s2][:, 16 + g * 8:24 + g * 8].unsqueeze(2).to_broadcast([128, 8, 64]), ALU.mult,
                 [t_ps[6], t_ncs[s2]], [t_y1])
            P.tt(y1[:, gs_], psum[:, 5, :], y1[:, gs_], ALU.add, [t_ps[5], t_y1], [t_y1])
        P.tt(y2[:, :], y1[:, :], sz[s2][:, :], ALU.mult, [t_y1, t_sz[s2]], [t_y2])
        P.act(scr_b[:, :], y2[:, :], AF.Square, [t_y2], [t_scrb, t_stb], accum_out=st_b[:, 0:1])
        rstd_act(P, st_b, 0, 1, 1.0 / 1024, t_stb)
        P.stt(yn[s2][:, :], y2[:, :], st_b[:, 1:2], gssd[:, :], ALU.mult, ALU.mult, [t_y2, t_stb, t_row], [t_yn[s2]])
        P.dma("sync", yssd_dst[tok, :], yn[s2][:, :], "a_y%d" % s2, [t_yn[s2]], [])

    if NFILL > 0:
        P.fill = (NFILL, psum[:, 4, 64:512], K.identB, K.mneg[:, 0:448])
    run_pipeline(chunk, NCH, 2 if state_only else 4, DEP)
    P.fill = None
    if state_only:
        P.dma("sync", st_out[0:128, 0:1024], S[:, :], "st_st", [t_S], [])
        P.dma("gpsimd", st_out[128:131, :], raw[125:128, :], "st_st2", [t_raw], [])


def run_pipeline(body, n, offset, depth=2):
    active = []
    nxt = 0
    while nxt < n or active:
        if nxt < n and (not active or (len(active) < depth and active[-1][1] >= offset)):
            active.append([body(nxt), 0])
            nxt += 1
        for a in list(active):
            try:
                next(a[0])
                a[1] += 1
            except StopIteration:
                active.remove(a)


def mixer_B(C, K, L, T_tok, h_src, h_dst, hnT_src, yssd_src, w_in, sgu_g, sgu_b, wsT_ap, bspT_ap, sguo_g,
            w_out, ffn_g, hnT2_dst, w_router, gates_dst):
    P = C.P
    NCH = T_tok // 128
    tg = "B%d_" % L
    sb = C.sb
    moe = w_router is not None
    win = sb(tg + "win", [128, 8, 2048], BF16)
    wout = sb(tg + "wout", [128, 16, 1024], BF16)
    rows = sb(tg + "rows", [128, 4, 1024], F32)
    wsf = sb(tg + "wsf", [128, 8, 128], F32)
    wsT = sb(tg + "wsT", [128, 8, 128], BF16)
    bsp = sb(tg + "bsp", [128, 8], F32)
    t_win, t_wout, t_rows, t_ws = T("win"), T("wout"), T("rows"), T("ws")
    for q in range(4):
        P.dma("gpsimd", win[:, :, q * 512:(q + 1) * 512],
              w_in[:, 2576 + q * 512:2576 + (q + 1) * 512].rearrange("(k p) f -> p k f", p=128), "cw_b_w", [], [t_win])
    for q in range(2):
        P.dma("gpsimd", wout[:, :, q * 512:(q + 1) * 512],
              w_out[:, q * 512:(q + 1) * 512].rearrange("(k p) f -> p k f", p=128), "cw_b_o", [], [t_wout])
    for i, r in enumerate((sgu_g, sgu_b, sguo_g, ffn_g)):
        P.dma("sync", rows[:, i, :], r.partition_broadcast(128), "cr_b_r", [], [t_rows])
    P.dma("sync", wsf[:, :, :], wsT_ap, "cr_b", [], [t_ws])
    P.dma("sync", bsp[:, :], bspT_ap, "cr_b", [], [t_ws])
    P.tt(wsT[:, :, :], wsf[:, :, :], K.triU.unsqueeze(1).to_broadcast([128, 8, 128]), ALU.mult, [t_ws, K.t], [t_ws])
    if moe:
        wr = sb(tg + "wr", [128, 8, 8], F32)
        wrh = sb(tg + "wrh", [128, 8, 8], BF16)
        wrl = sb(tg + "wrl", [128, 8, 8], BF16)
        t_wr = T("wr")
        P.dma("sync", wr[:, :, :], w_router, "cr_b2", [], [t_wr])
        P.copy(wrh[:, :, :], wr[:, :, :], [t_wr], [t_wr])
        P.tt(wrl[:, :, :], wr[:, :, :], wrh[:, :, :], ALU.subtract, [t_wr], [t_wr])
    gsgu, bsgu, gsguo, gffn = rows[:, 0, :], rows[:, 1, :], rows[:, 2, :], rows[:, 3, :]

    DEP = 3

    def two(name, shape, dt):
        return [sb(tg + name + "%d" % s, shape, dt) for s in range(DEP)]

    def twoT(name):
        return [T(name + "%d" % s) for s in range(DEP)]

    hin = two("hin", [128, 1024], F32)
    hnT = two("hnT", [128, 8, 128], BF16)
    ycat = two("ycat", [128, 2048], BF16)
    u = two("u", [128, 1024], BF16)
    v = sb(tg + "v", [128, 1024], F32)
    vn = sb(tg + "vn", [128, 1024], F32)
    vnb = sb(tg + "vnb", [128, 1024], BF16)
    scr_a = sb(tg + "scra", [128, 1024], BF16)
    scr_b = sb(tg + "scrb", [128, 1024], BF16)
    st_l = sb(tg + "stl", [128, 16], F32)
    st_a = sb(tg + "sta", [128, 4], F32)
    st_b = sb(tg + "stb", [128, 4], F32)
    t1 = sb(tg + "t1", [128, 1024], F32)
    ycT = sb(tg + "ycT", [128, 16, 128], BF16)
    hnew = two("hnew", [128, 1024], F32)
    hn2 = sb(tg + "hn2", [128, 1024], F32)
    hn2T = two("hn2T", [128, 8, 128], BF16)
    t_hin, t_hnT, t_ycat, t_u, t_hnew, t_hn2T = [twoT(n) for n in ("hin", "hnT", "ycat", "u", "hnew", "hn2T")]
    t_v, t_vn, t_vnb, t_scra, t_scrb, t_stl, t_sta, t_stb, t_t1, t_ycT, t_hn2 = [T(n) for n in (
        "v", "vn", "vnb", "scra", "scrb", "stl", "sta", "stb", "t1", "ycT", "hn2")]
    if moe:
        hn2Tl = sb(tg + "hn2Tl", [128, 8, 128], BF16)
        lg = two("lg", [128, 48], F32)
        t_hn2Tf = T("hn2Tl")
        t_lg = twoT("lg")
        gall = sb(tg + "gall", [128, NCH, 8], F32)
        t_gall = T("gall")
    psum, t_ps = C.psum, C.t_ps
    ps_tr = psum[:, 0:2, :].rearrange("p a (k t) -> p (a k) t", k=4)

    def chunk(c):
        s2 = c % DEP
        tok = slice(c * 128, (c + 1) * 128)
        for cc in ([0, 1] if c == 0 else [c + 1]):
            if cc < NCH:
                sc_ = cc % DEP
                tk = slice(cc * 128, (cc + 1) * 128)
                P.dma("sync", hnT[sc_][:, :, :], hnT_src[:, :, tk].rearrange("k p t -> p k t"), "b_hnT%d" % sc_, [],
                      [t_hnT[sc_]])
                P.dma("sync", ycat[sc_][:, 0:1024], yssd_src[tk, :], "b_y%d" % sc_, [], [t_ycat[sc_]])
                P.dma("sync", hin[sc_][:, :], h_src[tk, :], "b_h%d" % sc_, [], [t_hin[sc_]])
        for q in range(4):
            pb = q % 2
            for k in range(8):
                P.mm(psum[:, pb, :], hnT[s2][:, k, :], win[:, k, q * 512:(q + 1) * 512], k == 0, k == 7,
                     [t_hnT[s2], t_win], [t_ps[pb]])
            if q < 2:
                P.act(u[s2][:, q * 512:(q + 1) * 512], psum[:, pb, :], AF.Gelu, [t_ps[pb]], [t_u[s2]])
            else:
                P.act(v[:, (q - 2) * 512:(q - 1) * 512], psum[:, pb, :], AF.Gelu, [t_ps[pb]], [t_v])
        yield
        for q in range(2):
            P.op("vector", lambda e, q=q: e.bn_stats(st_l[:, q * 6:6 + q * 6], v[:, q * 512:(q + 1) * 512]),
                 [t_v], [t_stl])
        P.op("vector", lambda e: e.bn_aggr(st_l[:, 12:14], st_l[:, 0:12]), [t_stl], [t_stl])
        rstd_ops(P, st_l[:, 14:15], st_l[:, 13:14], 1.0, [t_stl], [t_stl])
        P.ts(vn[:, :], v[:, :], st_l[:, 12:13], st_l[:, 14:15], ALU.subtract, ALU.mult, [t_v, t_stl], [t_vn])
        P.tt(vn[:, :], vn[:, :], gsgu, ALU.mult, [t_vn, t_rows], [t_vn], eng="gpsimd")
        P.tt(vnb[:, :], vn[:, :], bsgu, ALU.add, [t_vn, t_rows], [t_vnb], eng="gpsimd")
        yield
        for h in range(8):
            pb = 6 + h // 4
            P.mm(psum[:, pb, (h % 4) * 128:(h % 4 + 1) * 128], wsT[:, h, :], vnb[:, h * 128:(h + 1) * 128], True, True,
                 [t_ws, t_vnb], [t_ps[pb]])
        P.tt(t1[:, :].rearrange("p (h d) -> p h d", h=8), psum[:, 6:8, :].rearrange("p a (h d) -> p (a h) d", h=4),
             bsp[:, :].unsqueeze(2).to_broadcast([128, 8, 128]), ALU.add, [t_ps[6], t_ps[7], t_ws], [t_t1])
        P.tt(t1[:, :], t1[:, :], u[s2][:, :], ALU.mult, [t_t1, t_u[s2]], [t_t1])
        P.act(scr_a[:, :], t1[:, :], AF.Square, [t_t1], [t_scra, t_sta], accum_out=st_a[:, 0:1])
        rstd_from_ss(P, st_a, 0, 1, 1.0 / 1024, t_sta)
        P.stt(ycat[s2][:, 1024:2048], t1[:, :], st_a[:, 1:2], gsguo, ALU.mult, ALU.mult, [t_t1, t_sta, t_rows],
              [t_ycat[s2]])
        yield
        ps_y = psum[:, 2:4, :].rearrange("p a b -> p (a b)").bitcast(BF16).rearrange("p (k t) -> p k t", k=16)
        for k in range(16):
            P.tr(ps_y[:, k, :], ycat[s2][:, k * 128:(k + 1) * 128], K.identB, [t_ycat[s2], K.t], [t_ps[2 + k // 8]])
        P.copy(ycT[:, :, :], ps_y, [t_ps[2], t_ps[3]], [t_ycT], eng="scalar")
        yield
        for hd in range(2):
            for k in range(16):
                P.mm(psum[:, 4 + hd, :], ycT[:, k, :], wout[:, k, hd * 512:(hd + 1) * 512], k == 0, k == 15,
                     [t_ycT, t_wout], [t_ps[4 + hd]])
        P.tt(hnew[s2][:, :].rearrange("p (a b) -> p a b", a=2), psum[:, 4:6, :],
             hin[s2][:, :].rearrange("p (a b) -> p a b", a=2), ALU.add, [t_ps[4], t_ps[5], t_hin[s2]], [t_hnew[s2]])
        P.dma("sync", h_dst[tok, :], hnew[s2][:, :], "b_ho%d" % s2, [t_hnew[s2]], [])
        yield
        P.act(scr_b[:, :], hnew[s2][:, :], AF.Square, [t_hnew[s2]], [t_scrb, t_stb], accum_out=st_b[:, 0:1])
        rstd_from_ss(P, st_b, 0, 1, 1.0 / 1024, t_stb)
        P.stt(hn2[:, :], hnew[s2][:, :], st_b[:, 1:2], gffn, ALU.mult, ALU.mult, [t_hnew[s2], t_stb, t_rows], [t_hn2])
        for k in range(8):
            P.tr(ps_tr[:, k, :], hn2[:, k * 128:(k + 1) * 128], K.identF, [t_hn2, K.t], [t_ps[k // 4]])
        P.copy(hn2T[s2][:, :, :], ps_tr, [t_ps[0], t_ps[1]], [t_hn2T[s2]], eng="scalar")
        P.dma("scalar", hnT2_dst[:, :, tok].rearrange("k p t -> p k t"), hn2T[s2][:, :, :], "b_h2T%d" % s2,
              [t_hn2T[s2]], [])
        if moe:
            l_ = lg[s2]
            tl = t_lg[s2]
            P.tt(hn2Tl[:, :, :], ps_tr, hn2T[s2][:, :, :], ALU.subtract, [t_ps[0], t_ps[1], t_hn2T[s2]], [t_hn2Tf])
            for k in range(8):
                P.mm(psum[:, 4, 0:8], hn2T[s2][:, k, :], wrh[:, k, :], k == 0, False, [t_hn2T[s2], t_wr], [t_ps[4]])
                P.mm(psum[:, 4, 0:8], hn2T[s2][:, k, :], wrl[:, k, :], False, False, [t_hn2T[s2], t_wr], [t_ps[4]])
                P.mm(psum[:, 4, 0:8], hn2Tl[:, k, :], wrh[:, k, :], False, k == 7, [t_hn2Tf, t_wr], [t_ps[4]])
            P.copy(l_[:, 0:8], psum[:, 4, 0:8], [t_ps[4]], [tl])
            P.op("vector", lambda e, l_=l_: e.max(l_[:, 8:16], l_[:, 0:8]), [tl], [tl])
            P.ts(l_[:, 16:24], l_[:, 0:8], l_[:, 9:10], None, ALU.is_ge, None, [tl], [tl])
            P.ts(l_[:, 40:41], l_[:, 8:9], -1.0, None, ALU.mult, None, [tl], [tl])
            P.act(l_[:, 24:32], l_[:, 0:8], AF.Exp, [tl], [tl], bias=l_[:, 40:41])
            P.tt(l_[:, 24:32], l_[:, 24:32], l_[:, 16:24], ALU.mult, [tl], [tl])
            P.op("vector", lambda e, l_=l_: e.tensor_reduce(l_[:, 41:42], l_[:, 24:32], mybir.AxisListType.X, ALU.add),
                 [tl], [tl])
            P.op("vector", lambda e, l_=l_: e.reciprocal(l_[:, 42:43], l_[:, 41:42]), [tl], [tl])
            P.ts(gall[:, c, :], l_[:, 24:32], l_[:, 42:43], None, ALU.mult, None, [tl], [t_gall])

    run_pipeline(chunk, NCH, 2, DEP)
    if moe:
        P.dma("sync", gates_dst, gall[:, :, :], "b_g", [t_gall], [])


D_IN_PROJ = 4624
NOMOE = False
NFILL = 0
MOE_STEPS = 3
PAIRS = [[0, 1], [2, 3], [4, 5], [6, 7]]
WNAMES = [("norm_mix_g", [2, 1024]), ("w_in", [2, 1024, D_IN_PROJ]), ("conv_w", [2, 4, 1536]), ("conv_b", [2, 1536]),
          ("dt_bias", [2, 16]), ("a_log", [2, 16]), ("d_skip", [2, 16]), ("ssd_norm_g", [2, 1024]),
          ("sgu_norm_g", [2, 1024]), ("sgu_norm_b", [2, 1024]), ("wsT", [2, 128, 8, 128]), ("bspT", [2, 128, 8]),
          ("sgu_out_g", [2, 1024]), ("w_out", [2, 2048, 1024]), ("norm_ffn_g", [2, 1024]),
          ("ffn_w_gate", [1, 1024, D_FF]), ("ffn_w_up", [1, 1024, D_FF]), ("ffn_w_down", [1, D_FF, 1024]),
          ("wr_l", [128, 8, 8]), ("moe_w_gate", [1, 8, 1024, D_FF]), ("moe_w_up", [1, 8, 1024, D_FF]),
          ("moe_w_down", [1, 8, D_FF, 1024]), ("final_norm_g", [1024])]


def build_program(T_tok=4096, TH=2048, layers=(0, 1), n_exp=N_EXPERTS, d_ff=D_FF, stop_after=None, dbg=False):
    nc = bass.Bass("TRN2", target_bir_lowering=False)
    I = {}
    I["x"] = nc.dram_tensor("x", [T_tok, 1024], F32, kind="ExternalInput").ap()
    for n, shp in WNAMES:
        shp = list(shp)
        if n in ("ffn_w_gate", "ffn_w_up"):
            shp[2] = d_ff
        if n == "ffn_w_down":
            shp[1] = d_ff
        if n in ("moe_w_gate", "moe_w_up"):
            shp[3] = d_ff
        if n == "moe_w_down":
            shp[2] = d_ff
        I[n] = nc.dram_tensor(n, shp, F32, kind="ExternalInput").ap()
    I["flag"] = nc.dram_tensor("flag", [128, 1], F32, kind="ExternalInput").ap()
    I["cf"] = nc.dram_tensor("cf", [128, 384], F32, kind="ExternalInput").ap()
    I["cb"] = nc.dram_tensor("cb", [128, 1792], BF16, kind="ExternalInput").ap()
    out = nc.dram_tensor("out", [T_tok, 1024], F32, kind="ExternalOutput").ap()
    hA = nc.dram_tensor("hA", [T_tok, 1024], F32).ap()
    hB = nc.dram_tensor("hB", [T_tok, 1024], F32).ap()
    hnTa = nc.dram_tensor("hnTa", [8, 128, T_tok], BF16).ap()
    hnTb = nc.dram_tensor("hnTb", [8, 128, T_tok], BF16).ap()
    yssd = nc.dram_tensor("yssd", [T_tok, 1024], BF16).ap()
    gates = nc.dram_tensor("gates", [128, T_tok // 128, 8], F32).ap()
    cc_in = [nc.dram_tensor("cc_in%d" % L, [131, 1536], F32).ap() for L in range(2)]
    cc_out = [nc.dram_tensor("cc_out%d" % L, [2 * 131, 1536], F32).ap() for L in range(2)]
    with contextlib.ExitStack() as gs:
        P = Prog(nc, gs)
        C = Ctx(nc, gs, P)
        C.psum = gs.enter_context(nc.psum_tensor("psum", [128, 8, 512], F32))
        C.t_ps = [T("ps%d" % b) for b in range(8)]
        K = load_consts(C, I["cf"], I["cb"])
        C.t_const = K.t

        def phase(fn):
            with contextlib.ExitStack() as ps:
                C.stack = ps
                C.t_ps = [T("ps%d" % b) for b in range(8)]
                fn()
                P.emit()

        def done(tag, src):
            return stop_after == tag

        stopped = False
        for L in layers:
            if stopped:
                break
            src = I["x"] if L == layers[0] else hB
            a_args = (I["w_in"][L], I["norm_mix_g"][L], I["conv_w"][L], I["conv_b"][L], I["dt_bias"][L],
                      I["a_log"][L], I["d_skip"][L], I["ssd_norm_g"][L])
            phase(lambda: mixer_A(C, K, L, T_tok, src, *a_args, True, None, cc_in[L], I["flag"], None, None))
            P.op("gpsimd", lambda e, L=L: e.collective_compute("AllGather", ALU.bypass, replica_groups=PAIRS,
                                                              ins=[cc_in[L]], outs=[cc_out[L]]),
                 [], [], dma_key="cc", inc=1)
            P.emit()
            phase(lambda: mixer_A(C, K, L, T_tok, src, *a_args, False, cc_out[L], None, I["flag"], hnTa, yssd))
            moe = (L % 2 == 1) and not NOMOE
            dstB = hA
            phase(lambda: mixer_B(C, K, L, T_tok, src, dstB, hnTa, yssd, I["w_in"][L], I["sgu_norm_g"][L],
                                  I["sgu_norm_b"][L], I["wsT"][L], I["bspT"][L], I["sgu_out_g"][L], I["w_out"][L],
                                  I["norm_ffn_g"][L], hnTb, I["wr_l"] if moe else None,
                                  gates if moe else None))
            if stop_after == "mix%d" % L:
                fin_dst, stopped = hA, True
                break
            last = (L == layers[-1]) and (L == 1)
            if not moe:
                wl = [(I["ffn_w_gate"][0], I["ffn_w_up"][0], I["ffn_w_down"][0], None)]
                gsrc = None
            else:
                wl = [(I["moe_w_gate"][0, e], I["moe_w_up"][0, e], I["moe_w_down"][0, e], e) for e in range(n_exp)]
                gsrc = gates
            dstF = out if last else hB
            phase(lambda: ffn_phase(C, "F%d_" % L, T_tok, TH, hA, hnTb, gsrc, wl, dstF,
                                    fin_g=I["final_norm_g"] if last else None))
            if stop_after == "ffn%d" % L and not last:
                fin_dst, stopped = hB, True
                break
        if stopped:
            def cp():
                tmp = C.sb("dbgcp", [128, T_tok // 128, 1024], F32)
                tt_ = T("dbg")
                P.dma("sync", tmp[:, :, :], fin_dst.rearrange("(j p) d -> p j d", p=128), "dbg", [], [tt_])
                P.dma("sync", out.rearrange("(j p) d -> p j d", p=128), tmp[:, :, :], "dbg", [tt_], [])
            phase(cp)
    return nc


_NC_CACHE = {}


def make_in_maps(inputs, n_cores=8, T_tok=4096):
    cf, cb = host_consts()
    x = np.asarray(inputs["x"], dtype=np.float32)
    B, S, D = x.shape
    halves = S // T_tok
    shared = {}
    for n, _ in WNAMES:
        if n == "wsT":
            shared[n] = np.ascontiguousarray(np.asarray(inputs["w_spatial"], np.float32).transpose(0, 3, 1, 2))
        elif n == "wr_l":
            shared[n] = np.ascontiguousarray(
                np.asarray(inputs["moe_w_router"], np.float32)[0].reshape(8, 128, 8).transpose(1, 0, 2))
        elif n == "bspT":
            shared[n] = np.ascontiguousarray(np.asarray(inputs["b_spatial"], np.float32).transpose(0, 2, 1))
        else:
            shared[n] = np.ascontiguousarray(np.asarray(inputs[n], np.float32))
    shared["cf"] = cf
    shared["cb"] = cb
    maps = []
    for c in range(n_cores):
        b, hf = c // halves, c % halves
        m = dict(shared)
        m["x"] = np.ascontiguousarray(x[b, hf * T_tok:(hf + 1) * T_tok])
        m["flag"] = np.full((128, 1), float(hf), np.float32)
        maps.append(m)
    return maps


def kernel(**inputs):
    if "full" not in _NC_CACHE:
        _NC_CACHE["full"] = build_program()
    nc = _NC_CACHE["full"]
    maps = make_in_maps(inputs)
    res = run_bass_kernel_spmd(nc, maps, core_ids=list(range(8)))
    x = inputs["x"]
    B, S, D = x.shape
    outp = np.empty((B, S, D), np.float32)
    for c in range(8):
        b, hf = c // 2, c % 2
        outp[b, hf * 4096:(hf + 1) * 4096] = res.results[c]["out"]
    return outp
```
